# Optimizing a Trainium2 kernel written in Bass

```python
import jax, jax.numpy as jnp
from jax import lax
import numpy as np

D_MODEL = 1024
BATCH = 4
SEQ = 4096
DEPTH = 2

EPS = 1e-6
NEG_INF = -1e30
HEAD_DIM = 64
ATT_HEADS = 8
ATT_WIDTH = ATT_HEADS * HEAD_DIM
MOBA_BLOCK = 256
MOBA_TOPK = 3
MOBA_Q_CHUNK = 32
ROPE_THETA = 10000.0
CONV_WIDTH = 256
CONV_GROUPS = 4
CONV_KERNEL = 31
GLA_HEADS = 4
GLA_DK = 64
GLA_DV = 64
GLA_KWIDTH = GLA_HEADS * GLA_DK
GLA_VWIDTH = GLA_HEADS * GLA_DV
GLA_GATE_RANK = 16
GLA_TAU = 16.0
GLA_CHUNK = 64
MIX_WIDTH = ATT_WIDTH + CONV_WIDTH + GLA_VWIDTH
IN_WIDTH = 3 * ATT_WIDTH + 2 * CONV_WIDTH + 2 * GLA_KWIDTH + 2 * GLA_VWIDTH + GLA_GATE_RANK
D_FF = 2816
FFN_RES = 0.5
N_MOD = 9

kernel_name = "hybrid_moba_conv_gla_macaron_adaln"


def _rmsnorm(x, gain):
    x32 = x.astype(jnp.float32)
    y = x32 * lax.rsqrt(jnp.mean(x32 * x32, axis=-1, keepdims=True) + EPS)
    return (y * gain.astype(jnp.float32)).astype(x.dtype)


def _layernorm(x, gain, bias):
    x32 = x.astype(jnp.float32)
    mu = jnp.mean(x32, axis=-1, keepdims=True)
    xc = x32 - mu
    y = xc * lax.rsqrt(jnp.mean(xc * xc, axis=-1, keepdims=True) + EPS)
    return (y * gain.astype(jnp.float32) + bias.astype(jnp.float32)).astype(x.dtype)


def _modulate(h, shift, scale):
    return h * (1 + scale[:, None, :]) + shift[:, None, :]


def _swiglu(h, w_in, w_out):
    a, b = jnp.split(h @ w_in, 2, axis=-1)
    return (jax.nn.silu(a) * b) @ w_out


def _split_heads(t, n_heads):
    bsz, seq, _ = t.shape
    return t.reshape(bsz, seq, n_heads, -1).transpose(0, 2, 1, 3)


def _merge_heads(t):
    bsz, n_h, seq, hd = t.shape
    return t.transpose(0, 2, 1, 3).reshape(bsz, seq, n_h * hd)


def _rope_tables(positions):
    inv_freq = 1.0 / (ROPE_THETA ** (jnp.arange(0, HEAD_DIM, 2, dtype=jnp.float32) / HEAD_DIM))
    ang = positions.astype(jnp.float32)[:, :, None] * inv_freq
    return jnp.cos(ang)[:, None], jnp.sin(ang)[:, None]


def _rope(t, cos, sin):
    t32 = t.astype(jnp.float32)
    t1, t2 = jnp.split(t32, 2, axis=-1)
    return jnp.concatenate([t1 * cos - t2 * sin, t2 * cos + t1 * sin], axis=-1).astype(t.dtype)


def _moba_attention(q, k, v):
    bsz, n_h, seq, hd = q.shape
    n_blk = -(-seq // MOBA_BLOCK)
    pad = n_blk * MOBA_BLOCK - seq
    widths = ((0, 0), (0, 0), (0, pad), (0, 0))
    kb = jnp.pad(k, widths).reshape(bsz, n_h, n_blk, MOBA_BLOCK, hd)
    vb = jnp.pad(v, widths).reshape(bsz, n_h, n_blk, MOBA_BLOCK, hd)
    k_mean = jnp.mean(kb.astype(jnp.float32), axis=3)
    gate = jnp.einsum('bhsd,bhnd->bhsn', q.astype(jnp.float32), k_mean)
    q_blk = jnp.arange(seq) // MOBA_BLOCK
    past = jnp.arange(n_blk)[None, :] < q_blk[:, None]
    gate = jnp.where(past, gate, NEG_INF)
    n_sel = min(MOBA_TOPK, n_blk)
    _, sel = lax.top_k(gate, n_sel)
    sel_ok = sel < q_blk[:, None]
    b_ix = jnp.arange(bsz)[:, None, None, None]
    h_ix = jnp.arange(n_h)[None, :, None, None]
    scale = hd ** -0.5
    qc_len = MOBA_Q_CHUNK

    def one_chunk(ci):
        t0 = ci * qc_len
        blk = t0 // MOBA_BLOCK
        qc = lax.dynamic_slice_in_dim(q, t0, qc_len, axis=2)
        sc = lax.dynamic_slice_in_dim(sel, t0, qc_len, axis=2)
        okc = lax.dynamic_slice_in_dim(sel_ok, t0, qc_len, axis=2)
        k_own = lax.dynamic_index_in_dim(kb, blk, axis=2, keepdims=False)
        v_own = lax.dynamic_index_in_dim(vb, blk, axis=2, keepdims=False)
        k_sel = kb[b_ix, h_ix, sc]
        v_sel = vb[b_ix, h_ix, sc]
        s_own = jnp.einsum('bhqd,bhld->bhql', qc, k_own).astype(jnp.float32) * scale
        q_pos = t0 + jnp.arange(qc_len)
        k_pos = blk * MOBA_BLOCK + jnp.arange(MOBA_BLOCK)
        s_own = jnp.where(k_pos[None, :] <= q_pos[:, None], s_own, NEG_INF)
        s_sel = jnp.einsum('bhqd,bhqnld->bhqnl', qc, k_sel).astype(jnp.float32) * scale
        s_sel = jnp.where(okc[..., None], s_sel, NEG_INF)
        s_all = jnp.concatenate([s_own, s_sel.reshape(bsz, n_h, qc_len, n_sel * MOBA_BLOCK)], axis=-1)
        p = jax.nn.softmax(s_all, axis=-1).astype(v.dtype)
        p_own = p[..., :MOBA_BLOCK]
        p_sel = p[..., MOBA_BLOCK:].reshape(bsz, n_h, qc_len, n_sel, MOBA_BLOCK)
        return (jnp.einsum('bhql,bhld->bhqd', p_own, v_own)
                + jnp.einsum('bhqnl,bhqnld->bhqd', p_sel, v_sel))

    out = lax.map(one_chunk, jnp.arange(seq // qc_len))
    return out.transpose(1, 2, 0, 3, 4).reshape(bsz, n_h, seq, hd)


def _conformer_conv(u, w_dw, b_dw, gain, bias):
    a, g = jnp.split(u, 2, axis=-1)
    h = a * jax.nn.sigmoid(g)
    h = lax.conv_general_dilated(h, w_dw[:, None, :], (1,), [(CONV_KERNEL - 1, 0)],
                                 dimension_numbers=('NWC', 'WIO', 'NWC'),
                                 feature_group_count=CONV_WIDTH) + b_dw
    bsz, seq, ch = h.shape
    hg = h.reshape(bsz, seq, CONV_GROUPS, ch // CONV_GROUPS)
    hg = _layernorm(hg, gain.reshape(CONV_GROUPS, -1), bias.reshape(CONV_GROUPS, -1))
    return jax.nn.silu(hg.reshape(bsz, seq, ch))


def _gla(q, k, v, g):
    bsz, n_h, seq, dk = q.shape
    dv = v.shape[-1]
    L = GLA_CHUNK
    n_c = seq // L
    f32 = jnp.float32
    qc = (q.astype(f32) * dk ** -0.5).reshape(bsz, n_h, n_c, L, dk)
    kc = k.astype(f32).reshape(bsz, n_h, n_c, L, dk)
    vc = v.astype(f32).reshape(bsz, n_h, n_c, L, dv)
    G = lax.cumsum(g.astype(f32).reshape(bsz, n_h, n_c, L, dk), axis=3)
    q_t = qc * jnp.exp(G)
    k_t = kc * jnp.exp(-G)
    causal = jnp.tril(jnp.ones((L, L), dtype=bool))
    A = jnp.where(causal, jnp.einsum('bhnld,bhnmd->bhnlm', q_t, k_t), 0.0)
    o_intra = jnp.einsum('bhnlm,bhnmv->bhnlv', A, vc)
    G_last = G[:, :, :, -1]
    k_end = kc * jnp.exp(G_last[:, :, :, None, :] - G)
    kv = jnp.einsum('bhnld,bhnlv->bhndv', k_end, vc)

    def step(state, inp):
        decay, kv_n = inp
        return decay[..., None] * state + kv_n, state

    init = jnp.zeros((bsz, n_h, dk, dv), f32)
    _, s_prev = lax.scan(step, init, (jnp.moveaxis(jnp.exp(G_last), 2, 0), jnp.moveaxis(kv, 2, 0)))
    s_prev = jnp.moveaxis(s_prev, 0, 2)
    o_inter = jnp.einsum('bhnld,bhndv->bhnlv', q_t, s_prev)
    return (o_intra + o_inter).reshape(bsz, n_h, seq, dv).astype(v.dtype)


def setup_inputs(seed: int = 0) -> dict:
    key = jax.random.key(seed)
    ks = jax.random.split(key, 24)
    f32 = jnp.float32

    def nrm(k, shape, s):
        return jax.random.normal(k, shape, f32) * s

    def gain(k, shape):
        return 1.0 + 0.05 * jax.random.normal(k, shape, f32)

    offset = jax.random.randint(ks[2], (BATCH, 1), 0, 1024, dtype=jnp.int32)
    positions = (offset + jnp.arange(SEQ, dtype=jnp.int32)[None, :]).astype(jnp.int32)
    return {
        "x": nrm(ks[0], (BATCH, SEQ, D_MODEL), 1.0),
        "c": nrm(ks[1], (BATCH, D_MODEL), 1.0),
        "positions": positions,
        "ada_w": nrm(ks[3], (DEPTH, D_MODEL, N_MOD * D_MODEL), 0.5 * D_MODEL ** -0.5),
        "ada_b": nrm(ks[4], (DEPTH, N_MOD * D_MODEL), 0.02),
        "ffn1_norm": gain(ks[5], (DEPTH, D_MODEL)),
        "ffn1_w_in": nrm(ks[6], (DEPTH, D_MODEL, 2 * D_FF), D_MODEL ** -0.5),
        "ffn1_w_out": nrm(ks[7], (DEPTH, D_FF, D_MODEL), D_FF ** -0.5),
        "mix_norm": gain(ks[8], (DEPTH, D_MODEL)),
        "mix_w_in": nrm(ks[9], (DEPTH, D_MODEL, IN_WIDTH), D_MODEL ** -0.5),
        "q_norm": gain(ks[10], (DEPTH, HEAD_DIM)),
        "k_norm": gain(ks[11], (DEPTH, HEAD_DIM)),
        "conv_w": nrm(ks[12], (DEPTH, CONV_KERNEL, CONV_WIDTH), CONV_KERNEL ** -0.5),
        "conv_b": nrm(ks[13], (DEPTH, CONV_WIDTH), 0.02),
        "conv_norm_g": gain(ks[14], (DEPTH, CONV_WIDTH)),
        "conv_norm_b": nrm(ks[15], (DEPTH, CONV_WIDTH), 0.02),
        "gla_gate_w": nrm(ks[16], (DEPTH, GLA_GATE_RANK, GLA_KWIDTH), GLA_GATE_RANK ** -0.5),
        "gla_gate_b": nrm(ks[17], (DEPTH, GLA_KWIDTH), 0.1),
        "gla_out_norm": gain(ks[18], (DEPTH, GLA_DV)),
        "mix_w_out": nrm(ks[19], (DEPTH, MIX_WIDTH, D_MODEL), MIX_WIDTH ** -0.5),
        "ffn2_norm": gain(ks[20], (DEPTH, D_MODEL)),
        "ffn2_w_in": nrm(ks[21], (DEPTH, D_MODEL, 2 * D_FF), D_MODEL ** -0.5),
        "ffn2_w_out": nrm(ks[22], (DEPTH, D_FF, D_MODEL), D_FF ** -0.5),
    }


def reference(x, c, positions, ada_w, ada_b, ffn1_norm, ffn1_w_in, ffn1_w_out, mix_norm, mix_w_in,
              q_norm, k_norm, conv_w, conv_b, conv_norm_g, conv_norm_b, gla_gate_w, gla_gate_b,
              gla_out_norm, mix_w_out, ffn2_norm, ffn2_w_in, ffn2_w_out):
    cos, sin = _rope_tables(positions)
    c_act = jax.nn.silu(c)
    splits = list(np.cumsum([ATT_WIDTH, ATT_WIDTH, ATT_WIDTH, 2 * CONV_WIDTH,
                             GLA_KWIDTH, GLA_KWIDTH, GLA_VWIDTH, GLA_VWIDTH]))
    for l in range(DEPTH):
        mod = c_act @ ada_w[l] + ada_b[l]
        sh1, sc1, gt1, sh2, sc2, gt2, sh3, sc3, gt3 = jnp.split(mod, N_MOD, axis=-1)

        h = _modulate(_rmsnorm(x, ffn1_norm[l]), sh1, sc1)
        x = x + FFN_RES * gt1[:, None, :] * _swiglu(h, ffn1_w_in[l], ffn1_w_out[l])

        h = _modulate(_rmsnorm(x, mix_norm[l]), sh2, sc2)
        proj = h @ mix_w_in[l]
        a_q, a_k, a_v, b_u, c_q, c_k, c_v, c_r, c_g = jnp.split(proj, splits, axis=-1)

        qa = _rope(_rmsnorm(_split_heads(a_q, ATT_HEADS), q_norm[l]), cos, sin)
        ka = _rope(_rmsnorm(_split_heads(a_k, ATT_HEADS), k_norm[l]), cos, sin)
        va = _split_heads(a_v, ATT_HEADS)
        o_a = _merge_heads(_moba_attention(qa, ka, va))

        o_b = _conformer_conv(b_u, conv_w[l], conv_b[l], conv_norm_g[l], conv_norm_b[l])

        log_decay = jax.nn.log_sigmoid(c_g @ gla_gate_w[l] + gla_gate_b[l]) / GLA_TAU
        o_c = _gla(_split_heads(c_q, GLA_HEADS), _split_heads(c_k, GLA_HEADS),
                   _split_heads(c_v, GLA_HEADS), _split_heads(log_decay, GLA_HEADS))
        o_c = _merge_heads(_rmsnorm(o_c, gla_out_norm[l])) * jax.nn.silu(c_r)

        mixed = jnp.concatenate([o_a, o_b, o_c], axis=-1) @ mix_w_out[l]
        x = x + gt2[:, None, :] * mixed

        h = _modulate(_rmsnorm(x, ffn2_norm[l]), sh3, sc3)
        x = x + FFN_RES * gt3[:, None, :] * _swiglu(h, ffn2_w_in[l], ffn2_w_out[l])
    return x
```

```python
import contextlib
import numpy as np
import ml_dtypes
import concourse.bass as bass
import concourse.mybir as mybir
from concourse.bass_utils import run_bass_kernel_spmd

F32 = mybir.dt.float32
BF16 = mybir.dt.bfloat16
I32 = mybir.dt.int32
AF = mybir.ActivationFunctionType
ALU = mybir.AluOpType
AX = mybir.AxisListType
NPBF = ml_dtypes.bfloat16

D = 1024
TOK = 2048
SEQ = 4096
DFF = 2816
NJ = 22
INW = 3088
EPS = 1e-6
NEGB = -30000.0

PV_AB, PV_N1, PV_N2, PV_N3, PV_C, PV_QN, PV_KN, PV_IF, PV_W = 0, 72, 80, 88, 96, 104, 105, 106, 112
PB_CW, PB_CB, PB_CG, PB_CBE, PB_GN, PB_W = 0, 31, 32, 33, 34, 40

SAME_ENGINE_SYNC = True


class Sched:
    def __init__(self, nc, es, n_sp=20, n_pool=12):
        self.nc = nc
        self.eng = {"pe": nc.tensor, "act": nc.scalar, "dve": nc.vector, "pool": nc.gpsimd, "sp": nc.sync}
        self.sem = {k: es.enter_context(nc.semaphore("sem_" + k)) for k in ("pe", "act", "dve", "pool")}
        self.cnt = {k: 0 for k in self.sem}
        self.lanes = {
            "sp": [{"sem": es.enter_context(nc.semaphore("ls%d" % i)), "cnt": 0} for i in range(n_sp)],
            "pool": [{"sem": es.enter_context(nc.semaphore("lp%d" % i)), "cnt": 0} for i in range(n_pool)],
        }
        self.rr = {"sp": 0, "pool": 0}
        self.waited = {}
        self.lastw = {}
        self.readers = {}
        self.nwaits = 0
        self.nops = 0

    def _semobj(self, key):
        if isinstance(key, str):
            return self.sem[key]
        return self.lanes[key[1]][key[2]]["sem"]

    def _wait(self, eng, key, val):
        if self.waited.get((eng, key), 0) >= val:
            return
        self.eng[eng].wait_ge(self._semobj(key), val)
        self.waited[(eng, key)] = val
        self.nwaits += 1

    def _deps(self, eng, reads, writes):
        raw = {}
        war = {}

        def add(d, tok):
            if tok is not None:
                d[tok[0]] = max(d.get(tok[0], 0), tok[1])

        for b in reads:
            add(raw, self.lastw.get(b))
        for b in writes:
            add(raw, self.lastw.get(b))
            for k, v in self.readers.get(b, {}).items():
                add(war, (k, v))
        for k, v in raw.items():
            if k == eng and (eng == "pe" or not SAME_ENGINE_SYNC):
                continue
            self._wait(eng, k, v)
        for k, v in war.items():
            if k == eng:
                continue
            self._wait(eng, k, v)

    def _commit(self, tok, reads, writes):
        for b in reads:
            r = self.readers.setdefault(b, {})
            r[tok[0]] = max(r.get(tok[0], 0), tok[1])
        for b in writes:
            self.lastw[b] = tok
            self.readers[b] = {}

    def op(self, eng, fn, reads=(), writes=()):
        self._deps(eng, reads, writes)
        ins = fn(self.eng[eng])
        self.cnt[eng] += 1
        ins.then_inc(self.sem[eng], 1)
        self._commit((eng, self.cnt[eng]), reads, writes)
        self.nops += 1

    def dma(self, q, out, in_, reads=(), writes=()):
        lanes = self.lanes[q]
        i = self.rr[q]
        self.rr[q] = (i + 1) % len(lanes)
        lane = lanes[i]
        key = ("lane", q, i)
        if lane["cnt"] > 0:
            self._wait(q, key, 16 * lane["cnt"])
        self._deps(q, reads, writes)
        ins = self.eng[q].dma_start(out=out, in_=in_)
        lane["cnt"] += 1
        ins.then_inc(lane["sem"], 16)
        self._commit((key, 16 * lane["cnt"]), reads, writes)
        self.nops += 1

    def barrier(self, engines=("pe", "act", "dve", "pool", "sp")):
        for e in engines:
            for k in self.sem:
                if k != e and self.cnt[k] > 0:
                    self._wait(e, k, self.cnt[k])
            for q in self.lanes:
                for i, lane in enumerate(self.lanes[q]):
                    if lane["cnt"] > 0:
                        self._wait(e, ("lane", q, i), 16 * lane["cnt"])

    def finish(self):
        self.barrier(engines=("sp",))


class Ctx:
    pass


_UNIQ = [0]


def _sbt(nc, name, shape, dt):
    _UNIQ[0] += 1
    return nc.sbuf_tensor("%s_u%d" % (name, _UNIQ[0]), shape, dt)


def _mk_psum(nc, es, S, n=8):
    banks = [es.enter_context(nc.psum_tensor("psb%d" % i, [128, 512], F32)) for i in range(n)]
    st = {"i": 0}

    def nxt(lo=0, hi=n):
        i = st["i"]
        if i < lo or i >= hi:
            i = lo
        st["i"] = i + 1 if i + 1 < hi else lo
        return banks[i], ("ps", i)

    return banks, nxt


def emit_norm_mod(S, C, xT, hT, acol, scol, ntok=TOK):
    nt = ntok // 512
    for tt in range(nt):
        ts = slice(tt * 512, (tt + 1) * 512)
        pn, pk = C.nxt()
        for k in range(8):
            sq = C.sq[k % 2]
            S.op("act", lambda e, sq=sq, k=k: e.activation(out=sq[:], in_=xT[:, k, ts], func=AF.Square),
                 reads=[("xT", k, tt)], writes=[("sq", k % 2)])
            S.op("pe", lambda e, sq=sq, k=k: e.matmul(pn[:], C.ones_ms[:], sq[:], start=(k == 0), stop=(k == 7)),
                 reads=[("sq", k % 2)], writes=[pk])
        rs = C.rs[tt % 2]
        S.op("act", lambda e: e.activation(out=rs[:], in_=pn[:], func=AF.Sqrt, bias=C.eps_col[:], scale=1.0),
             reads=[], writes=[pk, ("rs", tt % 2)])
        S.op("dve", lambda e: e.reciprocal(rs[:], rs[:]), reads=[], writes=[("rs", tt % 2)])
        for k in range(8):
            tmp = C.tmp[k % 2]
            S.op("dve", lambda e, tmp=tmp, k=k: e.tensor_tensor(tmp[:], xT[:, k, ts], rs[:], ALU.mult),
                 reads=[("xT", k, tt), ("rs", tt % 2)], writes=[("tmp", k % 2)])
            S.op("act", lambda e, tmp=tmp, k=k: e.activation(out=hT[:, k, ts], in_=tmp[:], func=AF.Identity,
                                                             bias=scol(k), scale=acol(k)),
                 reads=[("tmp", k % 2), "modT"], writes=[("hT", k, tt)])


def emit_ffn(S, C, nc, xT, hT, acol, scol, gcol, w_in, w_out, tag):
    emit_norm_mod(S, C, xT, hT, acol, scol)
    w_in_v = w_in.rearrange("(k p) c -> p k c", p=128)
    w_out_v = w_out.rearrange("(j p) c -> p j c", p=128)
    with contextlib.ExitStack() as es:
        gT = es.enter_context(_sbt(nc, "gT" + tag, [128, 11, TOK], BF16))
        win = [es.enter_context(_sbt(nc, "win%d%s" % (i, tag), [128, 8, 512], BF16)) for i in range(2)]
        wout = [es.enter_context(_sbt(nc, "wout%d%s" % (i, tag), [128, 11, 256], BF16)) for i in range(2)]
        sa = [es.enter_context(_sbt(nc, "sa%d%s" % (i, tag), [128, 512], F32)) for i in range(2)]
        wi_i = 0
        wo_i = 0
        sa_i = 0
        for half in range(2):
            pieces = [(0, 2), (2, 2), (4, 2), (6, 2), (8, 2), (10, 1)]
            for (j0, nj) in pieces:
                wb = win[wi_i % 2]
                wkey = ("win", wi_i % 2)
                wi_i += 1
                ca = (half * 11 + j0) * 128
                ncol = nj * 128
                S.dma("pool", wb[:, :, 0:ncol], w_in_v[:, :, ca:ca + ncol], writes=[wkey])
                S.dma("pool", wb[:, :, 256:256 + ncol], w_in_v[:, :, DFF + ca:DFF + ca + ncol], writes=[wkey])
                for jj in range(nj):
                    jl = j0 + jj
                    for tt in range(4):
                        ts = slice(tt * 512, (tt + 1) * 512)
                        pa, pak = C.nxt()
                        pb, pbk = C.nxt()
                        for k in range(8):
                            S.op("pe", lambda e, k=k: e.matmul(pa[:], wb[:, k, jj * 128:(jj + 1) * 128], hT[:, k, ts],
                                                               start=(k == 0), stop=(k == 7)),
                                 reads=[wkey, ("hT", k, tt)], writes=[pak])
                        for k in range(8):
                            S.op("pe", lambda e, k=k: e.matmul(pb[:], wb[:, k, 256 + jj * 128:256 + (jj + 1) * 128],
                                                               hT[:, k, ts], start=(k == 0), stop=(k == 7)),
                                 reads=[wkey, ("hT", k, tt)], writes=[pbk])
                        sb_ = sa[sa_i % 2]
                        sk = ("sa", sa_i % 2)
                        sa_i += 1
                        S.op("act", lambda e: e.activation(out=sb_[:], in_=pa[:], func=AF.Silu), writes=[pak, sk])
                        S.op("dve", lambda e: e.tensor_tensor(gT[:, jl, ts], sb_[:], pb[:], ALU.mult),
                             reads=[sk], writes=[pbk, ("gT", jl, tt)])
            for o in range(8):
                if o % 2 == 0:
                    wo = wout[wo_i % 2]
                    wok = ("wout", wo_i % 2)
                    wo_i += 1
                    S.dma("pool", wo[:], w_out_v[:, half * 11:(half + 1) * 11, o * 128:(o + 2) * 128], writes=[wok])
                oc = (o % 2) * 128
                for tt in range(4):
                    ts = slice(tt * 512, (tt + 1) * 512)
                    py, pyk = C.nxt()
                    for j in range(11):
                        S.op("pe", lambda e, j=j: e.matmul(py[:], wo[:, j, oc:oc + 128], gT[:, j, ts], start=(j == 0), stop=(j == 10)),
                             reads=[wok, ("gT", j, tt)], writes=[pyk])
                    S.op("dve", lambda e: e.scalar_tensor_tensor(out=xT[:, o, ts], in0=py[:], scalar=gcol(o), in1=xT[:, o, ts],
                                                                 op0=ALU.mult, op1=ALU.add),
                         reads=["modT"], writes=[pyk, ("xT", o, tt)])
        S.barrier()


def emit_consts_common(S, C, nc, es, dram):
    C.ones_ms = es.enter_context(_sbt(nc, "ones_ms", [128, 128], BF16))
    C.eps_col = es.enter_context(_sbt(nc, "eps_col", [128, 1], F32))
    C.sq = [es.enter_context(_sbt(nc, "sq%d" % i, [128, 512], BF16)) for i in range(2)]
    C.rs = [es.enter_context(_sbt(nc, "rs%d" % i, [128, 512], F32)) for i in range(2)]
    C.tmp = [es.enter_context(_sbt(nc, "tmp%d" % i, [128, 512], F32)) for i in range(2)]
    S.dma("sp", C.ones_ms[:], dram["c_ones"][:, :], writes=["ones_ms"])
    S.op("dve", lambda e: e.memset(C.eps_col[:], EPS), writes=["eps_col"])
    C.one_col = es.enter_context(_sbt(nc, "one_col", [128, 1], F32))
    S.op("dve", lambda e: e.memset(C.one_col[:], 1.0), writes=["one_col"])
    S.barrier()


def emit_mod_cols(S, C, nc, es, pv, modT):
    if not hasattr(C, "acols"):
        C.acols = es.enter_context(_sbt(nc, "acols", [128, 24], F32))
        C.hg = es.enter_context(_sbt(nc, "hg", [128, 16], F32))
    for i, (sc0, n0) in enumerate([(8, PV_N1), (32, PV_N2), (56, PV_N3)]):
        S.op("dve", lambda e, i=i, sc0=sc0, n0=n0: e.scalar_tensor_tensor(
            out=C.acols[:, i * 8:(i + 1) * 8], in0=modT[:, sc0:sc0 + 8], scalar=1.0, in1=pv[:, n0:n0 + 8],
            op0=ALU.add, op1=ALU.mult), reads=["modT", "pv"], writes=["acols"])
    S.op("dve", lambda e: e.tensor_scalar(C.hg[:, 0:8], modT[:, 16:24], 0.5, None, ALU.mult), reads=["modT"], writes=["hg"])
    S.op("dve", lambda e: e.tensor_scalar(C.hg[:, 8:16], modT[:, 64:72], 0.5, None, ALU.mult), reads=["modT"], writes=["hg"])
    S.barrier()


def emit_adaln(S, C, nc, pv, modT, ada_w):
    aw = ada_w.rearrange("(k p) c -> p k c", p=128)
    with contextlib.ExitStack() as es:
        cact = es.enter_context(_sbt(nc, "cact", [128, 8], BF16))
        awb = [es.enter_context(_sbt(nc, "awb%d" % i, [128, 8, 1024], BF16)) for i in range(2)]
        S.op("act", lambda e: e.activation(out=cact[:], in_=pv[:, PV_C:PV_C + 8], func=AF.Silu), reads=["pv"], writes=["cact"])
        pm, pmk = C.nxt()
        for g in range(9):
            wb = awb[g % 2]
            wk = ("awb", g % 2)
            for hh in range(2):
                S.dma("pool", wb[:, :, hh * 512:(hh + 1) * 512], aw[:, :, g * 1024 + hh * 512:g * 1024 + (hh + 1) * 512], writes=[wk])
            for jj in range(8):
                j = g * 8 + jj
                for k in range(8):
                    S.op("pe", lambda e, k=k, jj=jj, j=j: e.matmul(pm[:, j:j + 1], wb[:, k, jj * 128:(jj + 1) * 128], cact[:, k:k + 1],
                                                                 start=(k == 0), stop=(k == 7)),
                         reads=[wk, "cact"], writes=[pmk])
        S.op("dve", lambda e: e.tensor_tensor(modT[:, 0:72], pm[:, 0:72], pv[:, PV_AB:PV_AB + 72], ALU.add),
             reads=["pv"], writes=[pmk, "modT"])
        S.barrier()


def emit_rope_tables(S, C, nc, es, pv, pos_ap, ntok=TOK):
    C.cosT = es.enter_context(_sbt(nc, "cosT", [128, ntok], F32))
    C.sinT = es.enter_context(_sbt(nc, "sinT", [128, ntok], F32))
    with contextlib.ExitStack() as es2:
        pi_ = es2.enter_context(_sbt(nc, "pos_i", [128, ntok], I32))
        u = es2.enter_context(_sbt(nc, "rope_u", [128, ntok], F32))
        ni = es2.enter_context(_sbt(nc, "rope_ni", [128, ntok], I32))
        nf = es2.enter_context(_sbt(nc, "rope_nf", [128, ntok], F32))
        f2 = es2.enter_context(_sbt(nc, "rope_f2", [128, ntok], F32))
        S.dma("sp", pi_[:], pos_ap.broadcast_to([128, ntok]), writes=["pos_i"])
        S.op("dve", lambda e: e.tensor_copy(u[:], pi_[:]), reads=["pos_i"], writes=["u"])
        S.op("dve", lambda e: e.tensor_scalar(u[:], u[:], pv[:, PV_IF:PV_IF + 1], None, ALU.mult), reads=["pv"], writes=["u"])
        S.op("dve", lambda e: e.tensor_copy(ni[:], u[:]), reads=["u"], writes=["ni"])
        S.op("dve", lambda e: e.tensor_copy(nf[:], ni[:]), reads=["ni"], writes=["nf"])
        S.op("dve", lambda e: e.tensor_tensor(u[:], u[:], nf[:], ALU.subtract), reads=["nf"], writes=["u"])
        S.op("act", lambda e: e.activation(out=C.sinT[:], in_=u[:], func=AF.Sin, scale=2.0 * np.pi), reads=["u"], writes=["sinT"])
        S.op("dve", lambda e: e.tensor_scalar(f2[:], u[:], 0.25, None, ALU.add), reads=["u"], writes=["f2"])
        S.op("dve", lambda e: e.tensor_scalar(nf[:], f2[:], 0.5, None, ALU.is_gt), reads=["f2"], writes=["nf"])
        S.op("dve", lambda e: e.tensor_tensor(f2[:], f2[:], nf[:], ALU.subtract), reads=["nf"], writes=["f2"])
        S.op("act", lambda e: e.activation(out=C.cosT[:], in_=f2[:], func=AF.Sin, scale=2.0 * np.pi), reads=["f2"], writes=["cosT"])
        S.barrier()


def emit_proj(S, C, nc, xT, hT, pv, wmi, dram, OA, gwb_ap, pos_ap):
    wv = wmi.rearrange("(k p) c -> p k c", p=128)
    with contextlib.ExitStack() as es:
        def sb(name, shape, dt=F32):
            return es.enter_context(_sbt(nc, name, shape, dt))
        W0 = 1024
        w = sb("wmi_sb", [128, 8, INW - W0], BF16)
        tri = sb("tri_f", [128, 128], F32)
        gwb = sb("gwb_sb", [17, 256], F32)
        cgT = sb("cgT", [32, 512], F32)
        e_sb = sb("e_sb", [128, 256], F32)
        sp_sb = sb("sp_sb", [128, 4, 256], F32)
        enG = sb("enG", [128, 4, 256], F32)
        eGT = sb("eGT", [128, 2, 512], F32)
        enGT = sb("enGT", [128, 2, 512], F32)
        ge_sb = sb("ge_sb", [128, 2, 32], F32)
        st_bf = [sb("st_bf%d" % i, [128, 512], BF16) for i in range(3)]
        st_f = [sb("st_f%d" % i, [128, 512], F32) for i in range(2)]
        sg = sb("sg", [128, 512], F32)
        gkst = sb("gkst", [128, 4, 256], BF16)
        gvst = sb("gvst", [128, 4, 256], BF16)
        vst = sb("vst", [128, 4, 512], BF16)
        cnt = {"bf": 0, "f": 0, "x": 0}

        def nbf():
            i = cnt["bf"] % 3
            cnt["bf"] += 1
            return st_bf[i], ("st_bf", i)

        def nf_():
            i = cnt["f"] % 2
            cnt["f"] += 1
            return st_f[i], ("st_f", i)

        for (c0, c1) in [(1024, 2048), (2048, 3088)]:
            S.dma("pool", w[:, :, c0 - W0:c1 - W0], wv[:, :, c0:c1], writes=["wmi"])
        S.dma("sp", tri[:], dram["c_tri"][:, :], writes=["tri"])
        S.dma("sp", gwb[:], gwb_ap, writes=["gwb"])
        S.op("dve", lambda e: e.memset(cgT[:], 1.0), writes=["cgT"])

        def mm8(ps, psk, c0, ncol, ts, tt):
            for k in range(8):
                S.op("pe", lambda e, k=k: e.matmul(ps[0:ncol, :], w[:, k, c0 - W0:c0 - W0 + ncol], hT[:, k, ts], start=(k == 0), stop=(k == 7)),
                     reads=["wmi", ("hT", k, tt)], writes=[psk])

        for tt in range(4):
            ts = slice(tt * 512, (tt + 1) * 512)
            p, pk = C.nxt()
            mm8(p, pk, 3072, 16, ts, tt)
            S.op("act", lambda e: e.copy(cgT[0:16, :], p[0:16, :]), writes=[pk, "cgT"])
            for s in range(4):
                ss = slice(s * 128, (s + 1) * 128)
                pz, pzk = C.nxt()
                S.op("pe", lambda e: e.matmul(pz[:, 0:256], cgT[0:17, ss], gwb[0:17, :], start=True, stop=True),
                     reads=["cgT", "gwb"], writes=[pzk])
                S.op("act", lambda e: e.activation(out=e_sb[:], in_=pz[:, 0:256], func=AF.Exp, scale=-1.0), writes=[pzk, "e_sb"])
                S.op("act", lambda e: e.activation(out=sp_sb[:, s, :], in_=e_sb[:], func=AF.Ln, bias=C.one_col[:], scale=1.0),
                     reads=["e_sb"], writes=[("sp", s)])
                pG, pGk = C.nxt()
                S.op("pe", lambda e: e.matmul(pG[:, 0:256], tri[:], sp_sb[:, s, :], start=True, stop=True),
                     reads=["tri", ("sp", s)], writes=[pGk])
                S.op("act", lambda e: e.activation(out=enG[:, s, :], in_=pG[:, 0:256], func=AF.Exp, scale=-1.0),
                     writes=[pGk, ("enG", s)])
                for hp in range(2):
                    pT_, pTk = C.nxt()
                    S.op("pe", lambda e, hp=hp: e.matmul(pT_[:, 0:128], sp_sb[:, s, hp * 128:(hp + 1) * 128], tri[:], start=True, stop=True),
                         reads=["tri", ("sp", s)], writes=[pTk])
                    S.op("act", lambda e, hp=hp: e.activation(out=eGT[:, hp, ss], in_=pT_[:, 0:128], func=AF.Exp),
                         writes=[pTk, ("eGT", hp)])
                    S.op("act", lambda e, hp=hp: e.activation(out=enGT[:, hp, ss], in_=pT_[:, 0:128], func=AF.Exp, scale=-1.0),
                         writes=[pTk, ("enGT", hp)])
            for hp in range(2):
                S.op("act", lambda e, hp=hp: e.copy(ge_sb[:, hp, tt * 8:(tt + 1) * 8], eGT[:, hp, 63:512:64]),
                     reads=[("eGT", hp)], writes=["ge_sb"])
            for hp in range(2):
                p, pk = C.nxt()
                mm8(p, pk, 2048 + hp * 128, 128, ts, tt)
                st, sk = nbf()
                S.op("dve", lambda e, hp=hp: e.scalar_tensor_tensor(out=st[:], in0=p[:], scalar=0.125, in1=eGT[:, hp, :],
                                                                     op0=ALU.mult, op1=ALU.mult),
                     reads=[("eGT", hp)], writes=[pk, sk])
                S.dma("sp", OA.fm("GQ", hp, ts), st[:], reads=[sk])
                p, pk = C.nxt()
                mm8(p, pk, 2304 + hp * 128, 128, ts, tt)
                st, sk = nbf()
                S.op("dve", lambda e, hp=hp: e.tensor_tensor(st[:], p[:], enGT[:, hp, :], ALU.mult),
                     reads=[("enGT", hp)], writes=[pk, sk])
                S.dma("sp", OA.fm("GK", hp, ts), st[:], reads=[sk])
                p, pk = C.nxt()
                mm8(p, pk, 2816 + hp * 128, 128, ts, tt)
                st, sk = nbf()
                S.op("act", lambda e: e.activation(out=st[:], in_=p[:], func=AF.Silu), writes=[pk, sk])
                S.dma("sp", OA.fm("GR", hp, ts), st[:], reads=[sk])
            for s in range(4):
                tok = slice(tt * 512 + s * 128, tt * 512 + (s + 1) * 128)
                p, pk = C.nxt()
                for k in range(8):
                    S.op("pe", lambda e, k=k: e.matmul(p[:], hT[:, k, tok], w[:, k, 2304 - W0:2816 - W0], start=(k == 0), stop=(k == 7)),
                         reads=["wmi", ("hT", k, tt)], writes=[pk])
                S.op("dve", lambda e: e.tensor_tensor(gkst[:, s, :], p[:, 0:256], enG[:, s, :], ALU.mult),
                     reads=[("enG", s)], writes=[pk, "gkst"])
                S.op("act", lambda e: e.copy(gvst[:, s, :], p[:, 256:512]), writes=[pk, "gvst"])
            for hs in range(2):
                for s4 in range(4):
                    S.dma("sp", OA.tm2("GKT", tt, s4, hs), gkst[:, s4, hs * 128:(hs + 1) * 128], reads=["gkst"])
                    S.dma("sp", OA.tm2("GV", tt, s4, hs), gvst[:, s4, hs * 128:(hs + 1) * 128], reads=["gvst"])
            for s in range(4):
                tok = slice(tt * 512 + s * 128, tt * 512 + (s + 1) * 128)
                p, pk = C.nxt()
                for k in range(8):
                    S.op("pe", lambda e, k=k: e.matmul(p[:], hT[:, k, tok], w[:, k, 1024 - W0:1536 - W0], start=(k == 0), stop=(k == 7)),
                         reads=["wmi", ("hT", k, tt)], writes=[pk])
                S.op("act", lambda e: e.copy(vst[:, s, :], p[:]), writes=[pk, "vst"])
            for hs in range(2):
                for s4 in range(4):
                    S.dma("sp", OA.tm2("V", tt, s4, hs), vst[:, s4, hs * 256:(hs + 1) * 256], reads=["vst"])
            for cc in range(2):
                pa, pak = C.nxt()
                mm8(pa, pak, 1536 + cc * 128, 128, ts, tt)
                pg, pgk = C.nxt()
                mm8(pg, pgk, 1792 + cc * 128, 128, ts, tt)
                S.op("act", lambda e: e.activation(out=sg[:], in_=pg[:], func=AF.Sigmoid), writes=[pgk, "sg"])
                st, sk = nf_()
                S.op("dve", lambda e: e.tensor_tensor(st[:], pa[:], sg[:], ALU.mult), reads=["sg"], writes=[pak, sk])
                S.dma("sp", OA.fm("HC", cc, ts), st[:], reads=[sk])
        for hp in range(2):
            S.dma("sp", OA.fm("GE", hp, slice(0, 32)), ge_sb[:, hp, :], reads=["ge_sb"])
        S.barrier()
    with contextlib.ExitStack() as es:
        def sb(name, shape, dt=F32):
            return es.enter_context(_sbt(nc, name, shape, dt))
        emit_rope_tables(S, C, nc, es, pv, pos_ap)
        w = sb("wqk_sb", [128, 8, 1024], BF16)
        W0 = 0
        blk = sb("blk_bf", [128, 128], BF16)
        rot = sb("rot_f", [128, 128], F32)
        km_sb = sb("km_sb", [128, 4, 8], F32)
        st_bf = [sb("st2_bf%d" % i, [128, 512], BF16) for i in range(3)]
        tn = [sb("tn%d" % i, [128, 512], F32) for i in range(3)]
        uu = [sb("uu%d" % i, [128, 512], F32) for i in range(3)]
        ww = [sb("ww%d" % i, [128, 512], F32) for i in range(3)]
        rsq = [sb("rsq%d" % i, [128, 512], F32) for i in range(3)]
        sqb = [sb("sqb%d" % i, [128, 512], BF16) for i in range(3)]
        S.dma("pool", w[:, :, :], wv[:, :, 0:1024], writes=["wmi"])
        S.dma("sp", blk[:], dram["c_blk"][:, :], writes=["blk"])
        S.dma("sp", rot[:], dram["c_rot"][:, :], writes=["rot"])
        cnt = {"bf": 0, "x": 0}

        def nbf():
            i = cnt["bf"] % 3
            cnt["bf"] += 1
            return st_bf[i], ("st_bf", i)

        def mm8(ps, psk, c0, ncol, ts, tt):
            for k in range(8):
                S.op("pe", lambda e, k=k: e.matmul(ps[0:ncol, :], w[:, k, c0:c0 + ncol], hT[:, k, ts], start=(k == 0), stop=(k == 7)),
                     reads=["wmi", ("hT", k, tt)], writes=[psk])

        its = [(tt, which, hp) for tt in range(4) for which in range(2) for hp in range(4)]
        st8 = {}

        def stage1(n):
            tt, which, hp = its[n]
            ts = slice(tt * 512, (tt + 1) * 512)
            i = n % 3
            p, pk = C.nxt()
            mm8(p, pk, which * 512 + hp * 128, 128, ts, tt)
            S.op("act", lambda e: e.activation(out=sqb[i][:], in_=p[:], func=AF.Square), writes=[pk, ("sqb", i)])
            st8[n] = (p, pk)

        def stage2(n):
            tt, which, hp = its[n]
            i = n % 3
            p, pk = st8.pop(n)
            gain_col = pv[:, PV_QN + which:PV_QN + which + 1]
            pm, pmk = C.nxt()
            S.op("pe", lambda e: e.matmul(pm[:], blk[:], sqb[i][:], start=True, stop=True), reads=["blk", ("sqb", i)], writes=[pmk])
            S.op("act", lambda e: e.activation(out=rsq[i][:], in_=pm[:], func=AF.Sqrt, bias=C.eps_col[:], scale=1.0),
                 writes=[pmk, ("rsq", i)])
            S.op("dve", lambda e: e.reciprocal(rsq[i][:], rsq[i][:]), writes=[("rsq", i)])
            S.op("dve", lambda e: e.scalar_tensor_tensor(out=tn[i][:], in0=p[:], scalar=gain_col, in1=rsq[i][:],
                                                         op0=ALU.mult, op1=ALU.mult),
                 reads=["pv", ("rsq", i)], writes=[pk, ("tn", i)])

        def stage3(n):
            tt, which, hp = its[n]
            ts = slice(tt * 512, (tt + 1) * 512)
            i = n % 3
            pr, prk = C.nxt()
            S.op("pe", lambda e: e.matmul(pr[:], rot[:], tn[i][:], start=True, stop=True), reads=["rot", ("tn", i)], writes=[prk])
            S.op("pool", lambda e: e.tensor_tensor(uu[i][:], tn[i][:], C.cosT[:, ts], ALU.mult),
                 reads=[("tn", i), "cosT"], writes=[("uu", i)])
            S.op("dve", lambda e: e.tensor_tensor(ww[i][:], pr[:], C.sinT[:, ts], ALU.mult), reads=["sinT"], writes=[prk, ("ww", i)])
            S.op("dve", lambda e: e.tensor_tensor(uu[i][:], uu[i][:], ww[i][:], ALU.add), reads=[("ww", i)], writes=[("uu", i)])
            st, sk = nbf()
            S.op("act", lambda e: e.copy(st[:], uu[i][:]), reads=[("uu", i)], writes=[sk])
            name = "QT" if which == 0 else "KT"
            S.dma("sp", OA.fm(name, hp, ts), st[:], reads=[sk])
            if which == 1:
                S.op("dve", lambda e: e.tensor_reduce(out=km_sb[:, hp, tt * 2:(tt + 1) * 2],
                                                      in_=uu[i][:].rearrange("p (b t) -> p b t", b=2), axis=AX.X, op=ALU.add),
                     reads=[("uu", i)], writes=["km_sb"])

        NI = len(its)
        for n in range(NI + 2):
            if n < NI:
                stage1(n)
            if 0 <= n - 1 < NI:
                stage2(n - 1)
            if 0 <= n - 2 < NI:
                stage3(n - 2)
        for hp in range(4):
            S.dma("sp", OA.fm("KM", hp, slice(0, 8)), km_sb[:, hp, :], reads=["km_sb"])
        S.barrier()


def emit_conv(S, C, nc, pvB, din, IB, OB):
    with contextlib.ExitStack() as es:
        def sb(name, shape, dt=F32):
            return es.enter_context(_sbt(nc, name, shape, dt))
        hc = sb("hc", [128, 30 + SEQ], F32)
        acc = [sb("cacc%d" % i, [128, 512], F32) for i in range(2)]
        xc = [sb("cxc%d" % i, [128, 512], F32) for i in range(2)]
        sq = [sb("csq%d" % i, [128, 512], F32) for i in range(2)]
        sd = [sb("csd%d" % i, [128, 512], F32) for i in range(2)]
        ob = [sb("cob%d" % i, [128, 512], BF16) for i in range(2)]
        blkf = sb("blkf", [128, 128], F32)
        S.dma("sp", blkf[:], din["c_blkf"][:, :], writes=["blkf"])
        S.op("dve", lambda e: e.memset(hc[:, 0:30], 0.0), writes=[("hc", -1)])
        for tt in range(8):
            S.dma("sp", hc[:, 30 + tt * 512:30 + (tt + 1) * 512], IB.fm("sp", "HC", 0, 128, tt // 4, slice((tt % 4) * 512, (tt % 4 + 1) * 512)), writes=[("hc", tt)])
        yield
        for tt in range(8):
            i = tt % 2
            hk = [("hc", tt), ("hc", tt - 1)]
            a = acc[i]
            S.op("dve", lambda e: e.tensor_scalar(a[:], hc[:, tt * 512:tt * 512 + 512], pvB[:, PB_CW:PB_CW + 1], pvB[:, PB_CB:PB_CB + 1],
                                                  ALU.mult, ALU.add), reads=hk + ["pvB"], writes=[("cacc", i)])
            for j in range(1, 31):
                S.op("dve", lambda e, j=j: e.scalar_tensor_tensor(out=a[:], in0=hc[:, tt * 512 + j:tt * 512 + j + 512],
                                                                  scalar=pvB[:, PB_CW + j:PB_CW + j + 1], in1=a[:],
                                                                  op0=ALU.mult, op1=ALU.add),
                     reads=hk + ["pvB"], writes=[("cacc", i)])
                if j % 4 == 0:
                    yield
            pm, pmk = C.nxt(0, 6)
            S.op("pe", lambda e: e.matmul(pm[:], blkf[:], a[:], start=True, stop=True), reads=["blkf", ("cacc", i)], writes=[pmk])
            S.op("dve", lambda e: e.tensor_tensor(xc[i][:], a[:], pm[:], ALU.subtract), reads=[("cacc", i)], writes=[pmk, ("cxc", i)])
            S.op("act", lambda e: e.activation(out=sq[i][:], in_=xc[i][:], func=AF.Square), reads=[("cxc", i)], writes=[("csq", i)])
            pv_, pvk = C.nxt(0, 6)
            S.op("pe", lambda e: e.matmul(pv_[:], blkf[:], sq[i][:], start=True, stop=True), reads=["blkf", ("csq", i)], writes=[pvk])
            S.op("act", lambda e: e.activation(out=sd[i][:], in_=pv_[:], func=AF.Sqrt, bias=C.eps_col[:], scale=1.0),
                 writes=[pvk, ("csd", i)])
            S.op("dve", lambda e: e.reciprocal(sd[i][:], sd[i][:]), writes=[("csd", i)])
            S.op("dve", lambda e: e.tensor_tensor(xc[i][:], xc[i][:], sd[i][:], ALU.mult), reads=[("csd", i)], writes=[("cxc", i)])
            S.op("act", lambda e: e.activation(out=ob[i][:], in_=xc[i][:], func=AF.Silu, bias=pvB[:, PB_CBE:PB_CBE + 1],
                                               scale=pvB[:, PB_CG:PB_CG + 1]), reads=[("cxc", i), "pvB"], writes=[("cob", i)])
            S.dma("sp", OB(256, 128, tt * 512, 512), ob[i][:], reads=[("cob", i)])
            yield
        while not C.conv_release:
            yield
        S.barrier()


def emit_gla(S, C, nc, pvB, din, IB, OB):
    NT = SEQ // 128
    with contextlib.ExitStack() as es:
        def sb(name, shape, dt=F32):
            return es.enter_context(_sbt(nc, name, shape, dt))
        qh = [sb("gq%d" % h, [128, SEQ], BF16) for h in range(2)]
        ktT = sb("gktT", [128, SEQ], BF16)
        ktc = [sb("gktc%d" % c, [128, NT, 128], BF16) for c in range(2)]
        vpad = sb("gvpad", [128, NT, 2, 128], BF16)
        AT = sb("gAT", [128, NT, 2, 128], BF16)
        Sall = sb("gSall", [128, 65, 128], F32)
        Sb = sb("gSb", [128, 64, 128], BF16)
        ge = sb("gge", [128, 64], F32)
        gmask = sb("ggmask", [128, 128], BF16)
        bmask = sb("gbmask", [128, 128], F32)
        blkf = sb("gblkf", [128, 128], F32)
        og = [C.tmp[0]] * 2
        gsq = [C.tmp[1]] * 2
        grs = [C.rs[0]] * 2
        gr = [sb("ggr%d" % i, [128, 512], BF16) for i in range(2)]
        gout = [sb("gout%d" % i, [128, 512], BF16) for i in range(2)]
        for t_, nm in ((gmask, "c_gmask"), (bmask, "c_bmask"), (blkf, "c_blkf")):
            S.dma("sp", t_[:], din[nm][:, :], writes=[nm])
        for th in range(2):
            S.dma("sp", ge[:, th * 32:(th + 1) * 32], IB.fm("sp", "GE", 0, 128, th, slice(0, 32)), writes=["gge"])
        S.op("dve", lambda e: e.memset(qh[0][64:128, :], 0.0), writes=["gq0z"])
        S.op("dve", lambda e: e.memset(qh[1][0:64, :], 0.0), writes=["gq1z"])
        S.op("pool", lambda e: e.memset(ktc[0][64:128, :, :], 0.0), writes=["gktc0z"])
        S.op("pool", lambda e: e.memset(ktc[1][0:64, :, :], 0.0), writes=["gktc1z"])
        S.op("pool", lambda e: e.memset(vpad[:], 0.0), writes=["gvpad"])
        S.op("dve", lambda e: e.memset(Sall[:, 0, :], 0.0), writes=[("S", 0)])
        HT_ = NT // 2
        for th in range(2):
            tk = slice(th * TOK, (th + 1) * TOK)
            tl = slice(th * HT_, (th + 1) * HT_)
            S.dma("sp", qh[0][0:64, tk], IB.fm("sp", "GQ", 0, 64, th, slice(0, TOK)), writes=["gq0"])
            S.dma("sp", qh[1][64:128, tk], IB.fm("sp", "GQ", 64, 64, th, slice(0, TOK)), writes=["gq1"])
            S.dma("sp", ktT[:, tk], IB.fm("sp", "GK", 0, 128, th, slice(0, TOK)), writes=["gktT"])
            gkt_v = IB.tm("sp", "GKT", th).rearrange("(t c p) f -> c p t f", c=2, p=64)
            S.dma("sp", ktc[0][0:64, tl, :], gkt_v[0], writes=["gktc0"])
            S.dma("sp", ktc[1][64:128, tl, :], gkt_v[1], writes=["gktc1"])
            gv_v = IB.tm("sp", "GV", th).rearrange("(t p) f -> p t f", p=128)
            for h in range(2):
                S.dma("sp", vpad[:, tl, h, h * 64:(h + 1) * 64], gv_v[:, :, h * 64:(h + 1) * 64], reads=[], writes=["gvpad"])
        for t in range(NT):
            tsl = slice(t * 128, (t + 1) * 128)
            for c in range(2):
                n = 2 * t + c
                p, pk = C.nxt()
                for h2 in range(2):
                    S.op("pe", lambda e, h2=h2: e.matmul(p[:, 0:128], ktc[c][:, t, :], vpad[:, t, h2, :], start=(h2 == 0), stop=(h2 == 1)),
                         reads=["gktc%d" % c, "gktc%dz" % c, "gvpad"], writes=[pk])
                S.op("dve", lambda e: e.scalar_tensor_tensor(out=Sall[:, n + 1, :], in0=p[:, 0:128], scalar=ge[:, n:n + 1], in1=bmask[:],
                                                             op0=ALU.mult, op1=ALU.mult),
                     reads=["gge", "c_bmask"], writes=[pk, ("S", n + 1)])
            for h in range(2):
                p, pk = C.nxt()
                S.op("pe", lambda e: e.matmul(p[:, 0:128], ktT[:, tsl], qh[h][:, tsl], start=True, stop=True),
                     reads=["gktT", "gq%d" % h, "gq%dz" % h], writes=[pk])
                S.op("dve", lambda e: e.tensor_tensor(AT[:, t, h, :], p[:, 0:128], gmask[:], ALU.mult),
                     reads=["c_gmask"], writes=[pk, ("AT", t)])
        for n in range(64):
            S.op("dve", lambda e: e.scalar_tensor_tensor(out=Sall[:, n + 1, :], in0=Sall[:, n, :], scalar=ge[:, n:n + 1], in1=Sall[:, n + 1, :],
                                                         op0=ALU.mult, op1=ALU.add),
                 reads=[("S", n), "gge"], writes=[("S", n + 1)])
            if n % 16 == 15:
                g0 = n - 15
                S.op("act", lambda e: e.copy(Sb[:, g0:g0 + 16, :], Sall[:, g0:g0 + 16, :]),
                     reads=[("S", i) for i in range(g0, g0 + 16)], writes=[("Sb", g0 // 16)])
        for tt in range(8):
            i = tt % 2
            S.dma("sp", gr[i][:], IB.fm("sp", "GR", 0, 128, tt // 4, slice((tt % 4) * 512, (tt % 4 + 1) * 512)), writes=[("ggr", i)])
            po, pok = C.nxt()
            for s4 in range(4):
                t = tt * 4 + s4
                osl = slice(s4 * 128, (s4 + 1) * 128)
                S.op("pe", lambda e: e.matmul(po[:, osl], vpad[:, t, 0, :], AT[:, t, 0, :], start=True, stop=False),
                     reads=["gvpad", ("AT", t)], writes=[pok])
                S.op("pe", lambda e: e.matmul(po[:, osl], vpad[:, t, 1, :], AT[:, t, 1, :], start=False, stop=False),
                     reads=["gvpad", ("AT", t)], writes=[pok])
                for c in range(2):
                    n = 2 * t + c
                    for h2 in range(2):
                        S.op("pe", lambda e, h2=h2: e.matmul(po[:, s4 * 128 + c * 64:s4 * 128 + (c + 1) * 64], Sb[:, n, :],
                                                             qh[h2][:, t * 128 + c * 64:t * 128 + (c + 1) * 64], start=False,
                                                             stop=(c == 1 and h2 == 1)),
                             reads=[("Sb", n // 16), "gq%d" % h2, "gq%dz" % h2], writes=[pok])
            S.op("act", lambda e: e.copy(og[i][:], po[:]), writes=[pok, ("gog", 0)])
            S.op("act", lambda e: e.activation(out=gsq[i][:], in_=og[i][:], func=AF.Square), reads=[("gog", 0)], writes=[("gsq", 0)])
            pm, pmk = C.nxt()
            S.op("pe", lambda e: e.matmul(pm[:], blkf[:], gsq[i][:], start=True, stop=True), reads=["c_blkf", ("gsq", 0)], writes=[pmk])
            S.op("act", lambda e: e.activation(out=grs[i][:], in_=pm[:], func=AF.Sqrt, bias=C.eps_col[:], scale=1.0),
                 writes=[pmk, ("grs", 0)])
            S.op("dve", lambda e: e.reciprocal(grs[i][:], grs[i][:]), writes=[("grs", 0)])
            S.op("dve", lambda e: e.scalar_tensor_tensor(out=og[i][:], in0=og[i][:], scalar=pvB[:, PB_GN:PB_GN + 1], in1=grs[i][:],
                                                         op0=ALU.mult, op1=ALU.mult), reads=["pvB", ("grs", 0)], writes=[("gog", 0)])
            S.op("dve", lambda e: e.tensor_tensor(gout[i][:], og[i][:], gr[i][:], ALU.mult), reads=[("ggr", i), ("gog", 0)],
                 writes=[("gout", i)])
            S.dma("sp", OB(384, 128, tt * 512, 512), gout[i][:], reads=[("gout", i)])
        S.barrier()


def emit_attn(S, C, nc, pvB, din, IB, OB, bg=None):
    NCH = SEQ // 128
    with contextlib.ExitStack() as es:
        def sb(name, shape, dt=F32):
            return es.enter_context(_sbt(nc, name, shape, dt))
        qa = [sb("qa%d" % i, [80, SEQ], BF16) for i in range(2)]
        ka = [sb("ka%d" % i, [80, SEQ], BF16) for i in range(2)]
        va = [sb("va%d" % i, [128, NCH, 128], BF16) for i in range(2)]
        kmf = [sb("kmf%d" % i, [64, 16], F32) for i in range(2)]
        kmb = [sb("kmb%d" % i, [64, 16], BF16) for i in range(2)]
        G1 = sb("G1", [128, 24, 16], F32)
        G2 = sb("G2", [128, 24, 16], F32)
        GE_ = sb("GE_", [128, 24, 16], F32)
        gm = sb("gm", [128, 24], F32)
        PMk = sb("PMk", [128, 24, 16], F32)
        NMk = sb("NMk", [128, 24, 16], F32)
        b80a = sb("b80a", [128, 24, 80], F32)
        pT = [sb("pT%d" % i, [128, 512], BF16) for i in range(3)]
        rden = [sb("rden%d" % i, [128, 256], F32) for i in range(2)]
        ost = [sb("ost%d" % i, [64, 256], BF16) for i in range(2)]
        cmask = sb("cmask", [128, 512], BF16)
        ident = sb("ident", [128, 128], F32)
        S.dma("sp", cmask[:], din["c_cmask"][:, :], writes=["cmask"])
        S.dma("sp", ident[:], din["c_ident"][:, :], writes=["ident"])
        S.dma("sp", PMk[:], din["c_pastmask"].rearrange("p (t j) -> p t j", j=16), writes=["pmk"])
        S.dma("sp", NMk[:], din["c_negmask"].rearrange("p (t j) -> p t j", j=16), writes=["nmk"])
        S.op("dve", lambda e: e.memset(b80a[:], 0.0), writes=["b80a"])
        for i in range(2):
            S.dma("sp", ka[i][64:80, :], din["c_onehot"][:, :], writes=[("ka_oh", i)])
            S.op("dve", lambda e, i=i: e.memset(qa[i][64:80, 0:1024], 0.0), writes=[("qa_z", i)])
            S.op("pool", lambda e, i=i: e.memset(va[i][:], 1.0), writes=[("va", i)])
        cnt = {"pt": 0, "o": 0, "g": 0}
        PO = [6, 7]
        for h in range(4):
            hb = h % 2
            for th in range(2):
                tk = slice(th * TOK, (th + 1) * TOK)
                S.dma("pool", qa[hb][0:64, tk], IB.fm("pool", "QT", h * 64, 64, th, slice(0, TOK)), writes=[("qa", hb)])
                S.dma("pool", ka[hb][0:64, tk], IB.fm("pool", "KT", h * 64, 64, th, slice(0, TOK)), writes=[("ka", hb)])
                vv = IB.tm("pool", "V", th).rearrange("(c p) f -> p c f", p=128)
                for c4 in range(2):
                    S.dma("pool", va[hb][:, th * 16 + c4 * 8:th * 16 + (c4 + 1) * 8, 0:64], vv[:, c4 * 8:(c4 + 1) * 8, h * 64:(h + 1) * 64],
                          writes=[("va", hb)])
                S.dma("pool", kmf[hb][:, th * 8:(th + 1) * 8], IB.fm("pool", "KM", h * 64, 64, th, slice(0, 8)), writes=[("kmf", hb)])
            S.op("act", lambda e: e.mul(kmb[hb][:], kmf[hb][:], 1.0 / 256.0), reads=[("kmf", hb)], writes=[("kmb", hb)])
            pg, pgk = C.nxt(0, 6)
            for t in range(24):
                qs = slice((8 + t) * 128, (9 + t) * 128)
                S.op("pe", lambda e: e.matmul(pg[:, t * 16:(t + 1) * 16], qa[hb][0:64, qs], kmb[hb][0:64, :], start=True, stop=True),
                     reads=[("qa", hb), ("kmb", hb)], writes=[pgk])
            mb = lambda: gm[:].unsqueeze(2).broadcast_to([128, 24, 16])
            S.op("dve", lambda e: e.tensor_tensor(G1[:], pg[:, 0:384].rearrange("p (t j) -> p t j", j=16), PMk[:], ALU.add),
                 reads=["pmk"], writes=[pgk, "G1"])
            S.op("dve", lambda e: e.reduce_max(gm[:], G1[:], AX.X), reads=["G1"], writes=["gm"])
            S.op("dve", lambda e: e.tensor_tensor(GE_[:], G1[:], mb(), ALU.is_ge), reads=["G1", "gm"], writes=["GE"])
            S.op("dve", lambda e: e.scalar_tensor_tensor(out=G2[:], in0=GE_[:], scalar=-1.0e30, in1=G1[:], op0=ALU.mult, op1=ALU.add),
                 reads=["GE", "G1"], writes=["G2"])
            S.op("dve", lambda e: e.reduce_max(gm[:], G2[:], AX.X), reads=["G2"], writes=["gm"])
            S.op("dve", lambda e: e.tensor_tensor(GE_[:], G2[:], mb(), ALU.is_ge), reads=["G2", "gm"], writes=["GE"])
            S.op("dve", lambda e: e.scalar_tensor_tensor(out=G2[:], in0=GE_[:], scalar=-1.0e30, in1=G2[:], op0=ALU.mult, op1=ALU.add),
                 reads=["GE"], writes=["G2"])
            S.op("dve", lambda e: e.reduce_max(gm[:], G2[:], AX.X), reads=["G2"], writes=["gm"])
            S.op("dve", lambda e: e.tensor_tensor(GE_[:], G1[:], mb(), ALU.is_lt), reads=["G1", "gm"], writes=["GE"])
            S.op("dve", lambda e: e.tensor_tensor(b80a[:, :, 64:80], GE_[:], NMk[:], ALU.mult), reads=["GE", "nmk"], writes=["b80a"])
            for t4 in range(6):
                p2, p2k = C.nxt(0, 6)
                for u in range(4):
                    S.op("pe", lambda e: e.matmul(p2[0:80, u * 128:(u + 1) * 128], b80a[:, t4 * 4 + u, :], ident[:], start=True, stop=True),
                         reads=["b80a", "ident"], writes=[p2k])
                q0 = (8 + t4 * 4) * 128
                S.op("act", lambda e: e.copy(qa[hb][64:80, q0:q0 + 512], p2[64:80, 0:512]), writes=[p2k, ("qa", hb)])
            steps = [(i, j) for i in range(16) for j in range(i + 1)]

            def emit_scores(k):
                i, j = steps[k]
                qs = slice(i * 256, (i + 1) * 256)
                ps, psk = C.nxt(0, 6)
                for c in range(2):
                    ks = slice((2 * j + c) * 128, (2 * j + c + 1) * 128)
                    S.op("pe", lambda e: e.matmul(ps[:, c * 256:(c + 1) * 256], ka[hb][0:80, ks], qa[hb][0:80, qs], start=True, stop=True),
                         reads=[("ka", hb), ("ka_oh", hb), ("qa", hb), ("qa_z", hb)], writes=[psk])
                return ps, psk

            pending = emit_scores(0)
            for k, (i, j) in enumerate(steps):
                ps, psk = pending
                if k + 1 < len(steps):
                    pending = emit_scores(k + 1)
                if j == 0:
                    oi = cnt["o"] % 2
                    cnt["o"] += 1
                po = C.banks[PO[oi]]
                pok = ("ps", PO[oi])
                pi = cnt["pt"] % 3
                cnt["pt"] += 1
                S.op("act", lambda e: e.activation(out=pT[pi][:], in_=ps[:], func=AF.Exp, scale=0.125), writes=[psk, ("pT", pi)])
                if j == i:
                    S.op("dve", lambda e: e.tensor_tensor(pT[pi][:], pT[pi][:], cmask[:], ALU.mult), reads=["cmask"], writes=[("pT", pi)])
                for c in range(2):
                    S.op("pe", lambda e: e.matmul(po[:, 0:256], va[hb][:, 2 * j + c, :], pT[pi][:, c * 256:(c + 1) * 256],
                                                  start=(j == 0 and c == 0), stop=(j == i and c == 1)),
                         reads=[("va", hb), ("pT", pi)], writes=[pok])
                if j == i:
                    S.op("dve", lambda e: e.reciprocal(rden[oi][64:128, :], po[64:128, 0:256]), writes=[pok, ("rden", oi)])
                    S.op("dve", lambda e: e.tensor_tensor(ost[oi][:], po[0:64, 0:256], rden[oi][64:128, :], ALU.mult),
                         reads=[("rden", oi)], writes=[pok, ("ost", oi)])
                    S.dma("sp", OB(h * 64, 64, i * 256, 256), ost[oi][:], reads=[("ost", oi)])
                    if bg is not None:
                        for _ in range(1 if i < 8 else 2):
                            next(bg, None)
        S.barrier()


A_OUT_SPECS = {
    "x1T": ([D, TOK], F32), "modT": ([128, 72], F32),
    "QT": ([512, TOK], BF16), "KT": ([512, TOK], BF16), "KM": ([512, 8], F32), "V": ([TOK, 512], BF16),
    "HC": ([256, TOK], F32), "GQ": ([256, TOK], BF16), "GK": ([256, TOK], BF16), "GKT": ([TOK, 256], BF16),
    "GV": ([TOK, 256], BF16), "GR": ([256, TOK], BF16), "GE": ([256, 32], F32),
}
CONST_SPECS = {
    "c_ones": ([128, 128], BF16), "c_blk": ([128, 128], BF16), "c_rot": ([128, 128], F32), "c_tri": ([128, 128], F32),
    "c_onehot": ([16, SEQ], BF16), "c_cmask": ([128, 512], BF16), "c_gmask": ([128, 128], BF16),
    "c_bmask": ([128, 128], F32), "c_blkf": ([128, 128], F32), "c_ident": ([128, 128], F32),
    "c_pastmask": ([128, 384], F32), "c_negmask": ([128, 384], F32),
}


def make_consts():
    c = {}
    c["c_ones"] = np.full((128, 128), 1.0 / 1024.0, np.float32).astype(NPBF)
    blk = np.zeros((128, 128), np.float32)
    blk[:64, :64] = 1.0 / 64.0
    blk[64:, 64:] = 1.0 / 64.0
    c["c_blk"] = blk.astype(NPBF)
    c["c_blkf"] = blk.copy()
    rot = np.zeros((128, 128), np.float32)
    for p in range(128):
        d = p % 64
        if d < 32:
            rot[p + 32, p] = -1.0
        else:
            rot[p - 32, p] = 1.0
    c["c_rot"] = rot
    m = np.arange(128)[:, None]
    l = np.arange(128)[None, :]
    same = (m // 64) == (l // 64)
    c["c_tri"] = np.where(same & (m <= l), -1.0 / 16.0, 0.0).astype(np.float32)
    c["c_gmask"] = np.where(same & (m <= l), 1.0, 0.0).astype(np.float32).astype(NPBF)
    c["c_bmask"] = same.astype(np.float32)
    oh = np.zeros((16, SEQ), np.float32)
    for j in range(16):
        oh[j, j * 256:(j + 1) * 256] = 1.0
    c["c_onehot"] = oh.astype(NPBF)
    kk = np.arange(128)[:, None, None] + 128 * np.arange(2)[None, :, None]
    qq = np.arange(256)[None, None, :]
    c["c_cmask"] = (kk <= qq).astype(np.float32).reshape(128, 512).astype(NPBF)
    c["c_ident"] = np.eye(128, dtype=np.float32)
    pm = np.zeros((128, 24, 16), np.float32)
    nm = np.zeros((128, 24, 16), np.float32)
    for t in range(24):
        i = (8 + t) // 2
        pm[:, t, i:] = -1.0e30
        nm[:, t, :i] = NEGB
    c["c_pastmask"] = pm.reshape(128, 384)
    c["c_negmask"] = nm.reshape(128, 384)
    return c


def _decl_in(nc, dram, name, shape, dt):
    dram[name] = nc.dram_tensor(name, list(shape), dt, kind="ExternalInput").ap()


def _decl_out(nc, dram, name, shape, dt):
    dram[name] = nc.dram_tensor(name, list(shape), dt, kind="ExternalOutput").ap()


def _cols(v):
    return np.ascontiguousarray(np.asarray(v, np.float32).reshape(-1, 128).T)


def make_pv(inp, l, b):
    pv = np.zeros((128, PV_W), np.float32)
    pv[:, PV_AB:PV_AB + 72] = _cols(inp["ada_b"][l])
    pv[:, PV_N1:PV_N1 + 8] = _cols(inp["ffn1_norm"][l])
    pv[:, PV_N2:PV_N2 + 8] = _cols(inp["mix_norm"][l])
    pv[:, PV_N3:PV_N3 + 8] = _cols(inp["ffn2_norm"][l])
    pv[:, PV_C:PV_C + 8] = _cols(inp["c"][b])
    pv[:, PV_QN] = np.tile(np.asarray(inp["q_norm"][l], np.float32), 2)
    pv[:, PV_KN] = np.tile(np.asarray(inp["k_norm"][l], np.float32), 2)
    inv_freq = (1.0 / (np.float32(10000.0) ** (np.arange(0, 64, 2, dtype=np.float32) / np.float32(64)))).astype(np.float32)
    pv[:, PV_IF] = np.tile(inv_freq, 4).astype(np.float64) / (2.0 * np.pi)
    return pv


def emit_outproj(S, C, nc, xT, hT, modT, wmo, IC):
    with contextlib.ExitStack() as es:
        w = es.enter_context(_sbt(nc, "wmo_sb", [128, 8, D], BF16))
        for k in range(8):
            for tt in range(4):
                S.dma("sp", hT[:, k, tt * 512:(tt + 1) * 512], IC(k, slice(tt * 512, (tt + 1) * 512)), writes=[("hT", k, tt)])
        for kc in range(8):
            hh, q = kc // 4, kc % 4
            r0 = (hh * 256 + q * 128) if q < 2 else ((512 + hh * 128) if q == 2 else (768 + hh * 128))
            S.dma("pool", w[:, kc, :], wmo[r0:r0 + 128, :], writes=["wmo"])
        for tt in range(4):
            ts = slice(tt * 512, (tt + 1) * 512)
            for o in range(8):
                py, pyk = C.nxt()
                for kc in range(8):
                    S.op("pe", lambda e, kc=kc: e.matmul(py[:], w[:, kc, o * 128:(o + 1) * 128], hT[:, kc, ts], start=(kc == 0), stop=(kc == 7)),
                         reads=["wmo", ("hT", kc, tt)], writes=[pyk])
                S.op("dve", lambda e: e.scalar_tensor_tensor(out=xT[:, o, ts], in0=py[:], scalar=modT[:, 40 + o:41 + o], in1=xT[:, o, ts],
                                                             op0=ALU.mult, op1=ALU.add),
                     reads=["modT"], writes=[pyk, ("xT", o, tt)])
        S.barrier()


def make_pvB(inp, l, hh):
    pb = np.zeros((128, PB_W), np.float32)
    cs = slice(hh * 128, (hh + 1) * 128)
    pb[:, PB_CW:PB_CW + 31] = np.asarray(inp["conv_w"][l], np.float32)[:, cs].T
    pb[:, PB_CB] = inp["conv_b"][l][cs]
    pb[:, PB_CG] = inp["conv_norm_g"][l][cs]
    pb[:, PB_CBE] = inp["conv_norm_b"][l][cs]
    pb[:, PB_GN] = np.tile(np.asarray(inp["gla_out_norm"][l], np.float32), 2)
    return pb


BF_SEGS = [("QT", 256, TOK), ("KT", 256, TOK), ("GQ", 128, TOK), ("GK", 128, TOK), ("GR", 128, TOK),
           ("V", TOK, 256), ("GKT", TOK, 128), ("GV", TOK, 128)]
F_SEGS = [("KM", 256, 8), ("HC", 128, TOK), ("GE", 128, 32)]
W_SPECS = {"ada_w": [D, 9 * D], "w1i": [D, 2 * DFF], "w1o": [DFF, D], "wmi": [D, INW], "gwb": [17, 256],
           "wmo": [D, D], "w2i": [D, 2 * DFF], "w2o": [DFF, D]}


def _seg_off(segs):
    off, o = {}, 0
    for (n, r, w) in segs:
        off[n] = (o, r, w)
        o += r * w
    return off, o


BF_OFF, NBF = _seg_off(BF_SEGS)
F_OFF, NF = _seg_off(F_SEGS)


def _views(buf_bf, buf_f, n_lead):
    v = {}
    for off, buf in ((BF_OFF, buf_bf), (F_OFF, buf_f)):
        for n, (o, r, w) in off.items():
            v[n] = [buf[i, o:o + r * w].rearrange("(r w) -> r w", w=w) for i in range(n_lead)]
    return v


class XchgA:
    def __init__(self, V):
        self.V = V

    def fm(self, name, blk, ts):
        nb = self.V[name][0].shape[0] // 128
        hs, r0 = blk // nb, (blk % nb) * 128
        return self.V[name][hs][r0:r0 + 128, ts]

    def tm2(self, name, tt, s4, hs):
        t0 = tt * 512 + s4 * 128
        return self.V[name][hs][t0:t0 + 128, :]


class XchgB:
    def __init__(self, V):
        self.V = V

    def fm(self, q, name, r0, nrows, th, tsl):
        return self.V[name][th][r0:r0 + nrows, tsl]

    def tm(self, q, name, th):
        return self.V[name][th]


def _p128(ap):
    return ap.rearrange("(p m) -> p m", p=128)


def build_fused(n_layers=2):
    nc = bass.Bass("TRN2", target_bir_lowering=False, num_devices=8)
    dram = {}
    _decl_in(nc, dram, "xT", [D, TOK], F32)
    _decl_in(nc, dram, "pos", [1, TOK], I32)
    for l in range(n_layers):
        _decl_in(nc, dram, "pv%d" % l, [128, PV_W], F32)
        _decl_in(nc, dram, "pvB%d" % l, [128, PB_W], F32)
        for k, shp in W_SPECS.items():
            _decl_in(nc, dram, "%s%d" % (k, l), shp, F32)
    for k, (shp, dt) in CONST_SPECS.items():
        _decl_in(nc, dram, k, shp, dt)
    out = {}
    _decl_out(nc, out, "yT", [D, TOK], F32)
    XA_bf = [nc.dram_tensor("xa_bf_%d" % l, [2, 2, NBF], BF16, addr_space="Shared").ap() for l in range(n_layers)]
    XA_f = [nc.dram_tensor("xa_f_%d" % l, [2, 2, NF], F32, addr_space="Shared").ap() for l in range(n_layers)]
    XB = [nc.dram_tensor("xb_%d" % l, [2, 2, 512 * TOK], BF16, addr_space="Shared").ap() for l in range(n_layers)]
    LA_bf = nc.dram_tensor("la_bf", [2, NBF], BF16).ap()
    LA_f = nc.dram_tensor("la_f", [2, NF], F32).ap()
    LB_bf = nc.dram_tensor("lb_bf", [2, NBF], BF16).ap()
    LB_f = nc.dram_tensor("lb_f", [2, NF], F32).ap()
    LOT = nc.dram_tensor("lot", [2, 512 * TOK], BF16).ap()
    LC = nc.dram_tensor("lc", [2, 512 * TOK], BF16).ap()
    VA = _views(LA_bf, LA_f, 2)
    VB = _views(LB_bf, LB_f, 2)
    lot_v = [LOT[i, :].rearrange("(r w) -> r w", w=TOK) for i in range(2)]
    lc_v = [LC[i, :].rearrange("(r w) -> r w", w=TOK) for i in range(2)]
    with contextlib.ExitStack() as es:
        S = Sched(nc, es)
        C = Ctx()
        C.banks, C.nxt = _mk_psum(nc, es, S)
        par_sp = nc.sync.snap(nc.sync.partition_id() % 2, min_val=0, max_val=1)
        par_pl = nc.gpsimd.snap(nc.gpsimd.partition_id() % 2, min_val=0, max_val=1)
        xT = es.enter_context(_sbt(nc, "xT_sb", [128, 8, TOK], F32))
        modT = es.enter_context(_sbt(nc, "modT_sb", [128, 72], F32))
        C.acols = es.enter_context(_sbt(nc, "acols", [128, 24], F32))
        C.hg = es.enter_context(_sbt(nc, "hg", [128, 16], F32))
        pvs = [es.enter_context(_sbt(nc, "pv_sb", [128, PV_W], F32)) for l in range(n_layers)]
        pvBs = [es.enter_context(_sbt(nc, "pvB_sb", [128, PB_W], F32)) for l in range(n_layers)]
        xv = dram["xT"].rearrange("(k p) t -> p k t", p=128)
        for k in range(8):
            for tt in range(4):
                S.dma("sp", xT[:, k, tt * 512:(tt + 1) * 512], xv[:, k, tt * 512:(tt + 1) * 512], writes=[("xT", k, tt)])
        for l in range(n_layers):
            S.dma("sp", pvs[l][:], dram["pv%d" % l][:, :], writes=["pv"])
            S.dma("sp", pvBs[l][:], dram["pvB%d" % l][:, :], writes=["pvB"])
        emit_consts_common(S, C, nc, es, dram)
        for l in range(n_layers):
            pv, pvB = pvs[l], pvBs[l]
            W = lambda k: dram["%s%d" % (k, l)]
            with contextlib.ExitStack() as esA:
                hT = esA.enter_context(_sbt(nc, "hT_sb", [128, 8, TOK], BF16))
                emit_adaln(S, C, nc, pv, modT, W("ada_w"))
                emit_mod_cols(S, C, nc, es, pv, modT)
                emit_ffn(S, C, nc, xT, hT,
                         acol=lambda k: C.acols[:, k:k + 1], scol=lambda k: modT[:, k:k + 1], gcol=lambda k: C.hg[:, k:k + 1],
                         w_in=W("w1i"), w_out=W("w1o"), tag="1")
                emit_norm_mod(S, C, xT, hT, acol=lambda k: C.acols[:, 8 + k:9 + k], scol=lambda k: modT[:, 24 + k:25 + k])
                emit_proj(S, C, nc, xT, hT, pv, W("wmi"), dram, XchgA(VA), W("gwb")[:, :], dram["pos"][0:1, :])
                S.barrier()
            S.dma("sp", _p128(XA_bf[l][bass.ds(par_sp, 1)].squeeze(0).rearrange("h n -> (h n)")), _p128(LA_bf.rearrange("h n -> (h n)")))
            S.dma("sp", _p128(XA_f[l][bass.ds(par_sp, 1)].squeeze(0).rearrange("h n -> (h n)")), _p128(LA_f.rearrange("h n -> (h n)")))
            S.barrier()
            nc.all_core_barrier()
            for th in range(2):
                S.dma("pool", _p128(LB_bf[th, :]), _p128(XA_bf[l][th, bass.ds(par_pl, 1), :].squeeze(0)))
                S.dma("pool", _p128(LB_f[th, :]), _p128(XA_f[l][th, bass.ds(par_pl, 1), :].squeeze(0)))
            S.barrier()
            IB = XchgB(VB)

            def OB(r0, nrows, tok0, ntok):
                th, t0 = tok0 // TOK, tok0 % TOK
                return lot_v[th][r0:r0 + nrows, t0:t0 + ntok]

            emit_gla(S, C, nc, pvB, dram, IB, OB)
            C.conv_release = False
            cg = emit_conv(S, C, nc, pvB, dram, IB, OB)
            next(cg)
            emit_attn(S, C, nc, pvB, dram, IB, OB, bg=cg)
            C.conv_release = True
            for _ in cg:
                pass
            S.barrier()
            S.dma("sp", _p128(XB[l][bass.ds(par_sp, 1)].squeeze(0).rearrange("h n -> (h n)")), _p128(LOT.rearrange("h n -> (h n)")))
            S.barrier()
            nc.all_core_barrier()
            for hs in range(2):
                S.dma("pool", _p128(LC[hs, :]), _p128(XB[l][hs, bass.ds(par_pl, 1), :].squeeze(0)))
            S.barrier()
            with contextlib.ExitStack() as esC:
                hT = esC.enter_context(_sbt(nc, "hT_sb", [128, 8, TOK], BF16))

                def IC(k, tsl):
                    return lc_v[k // 4][(k % 4) * 128:(k % 4 + 1) * 128, tsl]

                emit_outproj(S, C, nc, xT, hT, modT, W("wmo"), IC)
                emit_ffn(S, C, nc, xT, hT,
                         acol=lambda k: C.acols[:, 16 + k:17 + k], scol=lambda k: modT[:, 48 + k:49 + k], gcol=lambda k: C.hg[:, 8 + k:9 + k],
                         w_in=W("w2i"), w_out=W("w2o"), tag="2")
        ov = out["yT"].rearrange("(k p) t -> p k t", p=128)
        for k in range(8):
            S.dma("sp", ov[:, k, :], xT[:, k, :], reads=[("xT", k, tt) for tt in range(4)])
        S.finish()
        print("fused: ops", S.nops, "waits", S.nwaits)
    return nc


def host_inputs_fused(inp, consts, n_layers=2):
    maps = []
    shared = {}
    for l in range(n_layers):
        shared["ada_w%d" % l] = np.ascontiguousarray(inp["ada_w"][l], dtype=np.float32)
        shared["w1i%d" % l] = np.ascontiguousarray(inp["ffn1_w_in"][l], dtype=np.float32)
        shared["w1o%d" % l] = np.ascontiguousarray(inp["ffn1_w_out"][l], dtype=np.float32)
        shared["wmi%d" % l] = np.ascontiguousarray(inp["mix_w_in"][l], dtype=np.float32)
        shared["gwb%d" % l] = np.ascontiguousarray(np.concatenate([inp["gla_gate_w"][l], inp["gla_gate_b"][l][None]], axis=0), dtype=np.float32)
        shared["wmo%d" % l] = np.ascontiguousarray(inp["mix_w_out"][l], dtype=np.float32)
        shared["w2i%d" % l] = np.ascontiguousarray(inp["ffn2_w_in"][l], dtype=np.float32)
        shared["w2o%d" % l] = np.ascontiguousarray(inp["ffn2_w_out"][l], dtype=np.float32)
    x = np.asarray(inp["x"], np.float32)
    for r in range(8):
        b, hh = r // 2, r % 2
        m = dict(shared)
        m["xT"] = np.ascontiguousarray(x[b, hh * TOK:(hh + 1) * TOK, :].T)
        m["pos"] = np.ascontiguousarray(inp["positions"][b, hh * TOK:(hh + 1) * TOK][None]).astype(np.int32)
        for l in range(n_layers):
            m["pv%d" % l] = make_pv(inp, l, b)
            m["pvB%d" % l] = make_pvB(inp, l, hh)
        m.update(consts)
        maps.append(m)
    return maps


FUSED = True
_CACHE = {}


def _common_decl(nc, dram, names):
    for k in names:
        _decl_in(nc, dram, k, *CONST_SPECS[k])


def build_uA():
    nc = bass.Bass("TRN2", target_bir_lowering=False)
    dram = {}
    _decl_in(nc, dram, "xT", [D, TOK], F32)
    _decl_in(nc, dram, "pos", [1, TOK], I32)
    _decl_in(nc, dram, "pv", [128, PV_W], F32)
    for k in ("ada_w", "w1i", "w1o", "wmi", "gwb"):
        _decl_in(nc, dram, k, W_SPECS[k], F32)
    _common_decl(nc, dram, ("c_ones", "c_blk", "c_rot", "c_tri"))
    out = {}
    _decl_out(nc, out, "x1T", [D, TOK], F32)
    _decl_out(nc, out, "modT", [128, 72], F32)
    _decl_out(nc, out, "la_bf", [2, NBF], BF16)
    _decl_out(nc, out, "la_f", [2, NF], F32)
    VA = _views(out["la_bf"], out["la_f"], 2)
    with contextlib.ExitStack() as es:
        S = Sched(nc, es)
        C = Ctx()
        C.banks, C.nxt = _mk_psum(nc, es, S)
        xT = es.enter_context(_sbt(nc, "xT_sb", [128, 8, TOK], F32))
        modT = es.enter_context(_sbt(nc, "modT_sb", [128, 72], F32))
        C.acols = es.enter_context(_sbt(nc, "acols", [128, 24], F32))
        C.hg = es.enter_context(_sbt(nc, "hg", [128, 16], F32))
        pv = es.enter_context(_sbt(nc, "pv_sb", [128, PV_W], F32))
        xv = dram["xT"].rearrange("(k p) t -> p k t", p=128)
        for k in range(8):
            for tt in range(4):
                S.dma("sp", xT[:, k, tt * 512:(tt + 1) * 512], xv[:, k, tt * 512:(tt + 1) * 512], writes=[("xT", k, tt)])
        S.dma("sp", pv[:], dram["pv"][:, :], writes=["pv"])
        emit_consts_common(S, C, nc, es, dram)
        with contextlib.ExitStack() as esA:
            hT = esA.enter_context(_sbt(nc, "hT_sb", [128, 8, TOK], BF16))
            emit_adaln(S, C, nc, pv, modT, dram["ada_w"])
            emit_mod_cols(S, C, nc, es, pv, modT)
            S.dma("sp", out["modT"][:, :], modT[:], reads=["modT"])
            emit_ffn(S, C, nc, xT, hT,
                     acol=lambda k: C.acols[:, k:k + 1], scol=lambda k: modT[:, k:k + 1], gcol=lambda k: C.hg[:, k:k + 1],
                     w_in=dram["w1i"], w_out=dram["w1o"], tag="1")
            ov = out["x1T"].rearrange("(k p) t -> p k t", p=128)
            for k in range(8):
                S.dma("sp", ov[:, k, :], xT[:, k, :], reads=[("xT", k, tt) for tt in range(4)])
            emit_norm_mod(S, C, xT, hT, acol=lambda k: C.acols[:, 8 + k:9 + k], scol=lambda k: modT[:, 24 + k:25 + k])
            emit_proj(S, C, nc, xT, hT, pv, dram["wmi"], dram, XchgA(VA), dram["gwb"][:, :], dram["pos"][0:1, :])
        S.finish()
    return nc


def build_uB():
    nc = bass.Bass("TRN2", target_bir_lowering=False)
    dram = {}
    _decl_in(nc, dram, "lb_bf", [2, NBF], BF16)
    _decl_in(nc, dram, "lb_f", [2, NF], F32)
    _decl_in(nc, dram, "pvB", [128, PB_W], F32)
    _common_decl(nc, dram, ("c_onehot", "c_cmask", "c_gmask", "c_bmask", "c_blkf", "c_ident", "c_pastmask", "c_negmask"))
    out = {}
    _decl_out(nc, out, "lot", [2, 512 * TOK], BF16)
    VB = _views(dram["lb_bf"], dram["lb_f"], 2)
    lot_v = [out["lot"][i, :].rearrange("(r w) -> r w", w=TOK) for i in range(2)]
    with contextlib.ExitStack() as es:
        S = Sched(nc, es)
        C = Ctx()
        C.banks, C.nxt = _mk_psum(nc, es, S)
        pvB = es.enter_context(_sbt(nc, "pvB_sb", [128, PB_W], F32))
        C.eps_col = es.enter_context(_sbt(nc, "eps_col", [128, 1], F32))
        C.rs = [es.enter_context(_sbt(nc, "rs%d" % i, [128, 512], F32)) for i in range(2)]
        C.tmp = [es.enter_context(_sbt(nc, "tmp%d" % i, [128, 512], F32)) for i in range(2)]
        S.dma("sp", pvB[:], dram["pvB"][:, :], writes=["pvB"])
        S.op("dve", lambda e: e.memset(C.eps_col[:], EPS), writes=["eps_col"])
        S.barrier()
        IB = XchgB(VB)

        def OB(r0, nrows, tok0, ntok):
            th, t0 = tok0 // TOK, tok0 % TOK
            return lot_v[th][r0:r0 + nrows, t0:t0 + ntok]

        emit_gla(S, C, nc, pvB, dram, IB, OB)
        C.conv_release = False
        cg = emit_conv(S, C, nc, pvB, dram, IB, OB)
        next(cg)
        emit_attn(S, C, nc, pvB, dram, IB, OB, bg=cg)
        C.conv_release = True
        for _ in cg:
            pass
        S.finish()
    return nc


def build_uC():
    nc = bass.Bass("TRN2", target_bir_lowering=False)
    dram = {}
    _decl_in(nc, dram, "x1T", [D, TOK], F32)
    _decl_in(nc, dram, "modT", [128, 72], F32)
    _decl_in(nc, dram, "lc", [2, 512 * TOK], BF16)
    _decl_in(nc, dram, "pv", [128, PV_W], F32)
    for k in ("wmo", "w2i", "w2o"):
        _decl_in(nc, dram, k, W_SPECS[k], F32)
    _common_decl(nc, dram, ("c_ones",))
    out = {}
    _decl_out(nc, out, "x3T", [D, TOK], F32)
    lc_v = [dram["lc"][i, :].rearrange("(r w) -> r w", w=TOK) for i in range(2)]
    with contextlib.ExitStack() as es:
        S = Sched(nc, es)
        C = Ctx()
        C.banks, C.nxt = _mk_psum(nc, es, S)
        xT = es.enter_context(_sbt(nc, "xT_sb", [128, 8, TOK], F32))
        modT = es.enter_context(_sbt(nc, "modT_sb", [128, 72], F32))
        C.acols = es.enter_context(_sbt(nc, "acols", [128, 24], F32))
        C.hg = es.enter_context(_sbt(nc, "hg", [128, 16], F32))
        pv = es.enter_context(_sbt(nc, "pv_sb", [128, PV_W], F32))
        xv = dram["x1T"].rearrange("(k p) t -> p k t", p=128)
        for k in range(8):
            for tt in range(4):
                S.dma("sp", xT[:, k, tt * 512:(tt + 1) * 512], xv[:, k, tt * 512:(tt + 1) * 512], writes=[("xT", k, tt)])
        S.dma("sp", pv[:], dram["pv"][:, :], writes=["pv"])
        S.dma("sp", modT[:], dram["modT"][:, :], writes=["modT"])
        emit_consts_common(S, C, nc, es, dram)
        emit_mod_cols(S, C, nc, es, pv, modT)
        with contextlib.ExitStack() as esC:
            hT = esC.enter_context(_sbt(nc, "hT_sb", [128, 8, TOK], BF16))

            def IC(k, tsl):
                return lc_v[k // 4][(k % 4) * 128:(k % 4 + 1) * 128, tsl]

            emit_outproj(S, C, nc, xT, hT, modT, dram["wmo"], IC)
            emit_ffn(S, C, nc, xT, hT,
                     acol=lambda k: C.acols[:, 16 + k:17 + k], scol=lambda k: modT[:, 48 + k:49 + k], gcol=lambda k: C.hg[:, 8 + k:9 + k],
                     w_in=dram["w2i"], w_out=dram["w2o"], tag="2")
        ov = out["x3T"].rearrange("(k p) t -> p k t", p=128)
        for k in range(8):
            S.dma("sp", ov[:, k, :], xT[:, k, :], reads=[("xT", k, tt) for tt in range(4)])
        S.finish()
    return nc


def kernel_unfused(inp, consts, trace=False):
    if "uA" not in _CACHE:
        _CACHE["uA"], _CACHE["uB"], _CACHE["uC"] = build_uA(), build_uB(), build_uC()
    cores = list(range(8))
    maps = host_inputs_fused(inp, consts, 2)
    xT_cores = [m["xT"] for m in maps]
    times = []
    for l in range(2):
        inA = [{"xT": xT_cores[r], "pos": maps[r]["pos"], "pv": maps[r]["pv%d" % l],
                **{k: maps[r]["%s%d" % (k, l)] for k in ("ada_w", "w1i", "w1o", "wmi", "gwb")},
                **{k: consts[k] for k in ("c_ones", "c_blk", "c_rot", "c_tri")}} for r in cores]
        rA = run_bass_kernel_spmd(_CACHE["uA"], inA, core_ids=cores, trace=trace)
        times.append(("A%d" % l, rA.exec_time_ns))
        rA = rA.results
        inB = [{"lb_bf": np.ascontiguousarray(np.stack([rA[2 * (r // 2) + th]["la_bf"][r % 2] for th in range(2)])),
                "lb_f": np.ascontiguousarray(np.stack([rA[2 * (r // 2) + th]["la_f"][r % 2] for th in range(2)])),
                "pvB": maps[r]["pvB%d" % l],
                **{k: consts[k] for k in ("c_onehot", "c_cmask", "c_gmask", "c_bmask", "c_blkf", "c_ident", "c_pastmask", "c_negmask")}}
               for r in cores]
        rB = run_bass_kernel_spmd(_CACHE["uB"], inB, core_ids=cores, trace=trace)
        times.append(("B%d" % l, rB.exec_time_ns))
        rB = rB.results
        inC = [{"x1T": rA[r]["x1T"], "modT": rA[r]["modT"],
                "lc": np.ascontiguousarray(np.stack([rB[2 * (r // 2) + hs]["lot"][r % 2] for hs in range(2)])),
                "pv": maps[r]["pv%d" % l],
                **{k: maps[r]["%s%d" % (k, l)] for k in ("wmo", "w2i", "w2o")}, "c_ones": consts["c_ones"]} for r in cores]
        rC = run_bass_kernel_spmd(_CACHE["uC"], inC, core_ids=cores, trace=trace)
        times.append(("C%d" % l, rC.exec_time_ns))
        xT_cores = [np.ascontiguousarray(rC.results[r]["x3T"]) for r in cores]
    if trace:
        print("PHASE TIMES", times)
    return xT_cores


def kernel(**inp):
    inp = {k: np.asarray(v) for k, v in inp.items()}
    consts = make_consts()
    out = np.empty((4, SEQ, D), np.float32)
    if FUSED:
        if "nc" not in _CACHE:
            _CACHE["nc"] = build_fused(2)
        res = run_bass_kernel_spmd(_CACHE["nc"], host_inputs_fused(inp, consts, 2), core_ids=list(range(8))).results
        for r in range(8):
            out[r // 2, (r % 2) * TOK:(r % 2 + 1) * TOK, :] = res[r]["yT"].T
    else:
        xs = kernel_unfused(inp, consts)
        for r in range(8):
            out[r // 2, (r % 2) * TOK:(r % 2 + 1) * TOK, :] = xs[r].T
    return out
```

```python
import contextlib
import numpy as np
import ml_dtypes
import concourse.bass as bass
import concourse.mybir as mybir
from concourse.bass_utils import run_bass_kernel_spmd

F32 = mybir.dt.float32
BF16 = mybir.dt.bfloat16
I32 = mybir.dt.int32
AF = mybir.ActivationFunctionType
ALU = mybir.AluOpType
AX = mybir.AxisListType
NPBF = ml_dtypes.bfloat16

D = 1024
TOK = 2048
SEQ = 4096
DFF = 2816
NJ = 22
INW = 3088
EPS = 1e-6
NEGB = -30000.0

PV_AB, PV_N1, PV_N2, PV_N3, PV_C, PV_QN, PV_KN, PV_IF, PV_W = 0, 72, 80, 88, 96, 104, 105, 106, 112
PB_CW, PB_CB, PB_CG, PB_CBE, PB_GN, PB_W = 0, 31, 32, 33, 34, 40

SAME_ENGINE_SYNC = True


class Sched:
    def __init__(self, nc, es, n_sp=20, n_pool=12):
        self.nc = nc
        self.eng = {"pe": nc.tensor, "act": nc.scalar, "dve": nc.vector, "pool": nc.gpsimd, "sp": nc.sync}
        self.sem = {k: es.enter_context(nc.semaphore("sem_" + k)) for k in ("pe", "act", "dve", "pool")}
        self.cnt = {k: 0 for k in self.sem}
        self.lanes = {
            "sp": [{"sem": es.enter_context(nc.semaphore("ls%d" % i)), "cnt": 0} for i in range(n_sp)],
            "pool": [{"sem": es.enter_context(nc.semaphore("lp%d" % i)), "cnt": 0} for i in range(n_pool)],
        }
        self.rr = {"sp": 0, "pool": 0}
        self.waited = {}
        self.lastw = {}
        self.readers = {}
        self.nwaits = 0
        self.nops = 0

    def _semobj(self, key):
        if isinstance(key, str):
            return self.sem[key]
        return self.lanes[key[1]][key[2]]["sem"]

    def _wait(self, eng, key, val):
        if self.waited.get((eng, key), 0) >= val:
            return
        self.eng[eng].wait_ge(self._semobj(key), val)
        self.waited[(eng, key)] = val
        self.nwaits += 1

    def _deps(self, eng, reads, writes):
        raw = {}
        war = {}

        def add(d, tok):
            if tok is not None:
                d[tok[0]] = max(d.get(tok[0], 0), tok[1])

        for b in reads:
            add(raw, self.lastw.get(b))
        for b in writes:
            add(raw, self.lastw.get(b))
            for k, v in self.readers.get(b, {}).items():
                add(war, (k, v))
        for k, v in raw.items():
            if k == eng and (eng == "pe" or not SAME_ENGINE_SYNC):
                continue
            self._wait(eng, k, v)
        for k, v in war.items():
            if k == eng:
                continue
            self._wait(eng, k, v)

    def _commit(self, tok, reads, writes):
        for b in reads:
            r = self.readers.setdefault(b, {})
            r[tok[0]] = max(r.get(tok[0], 0), tok[1])
        for b in writes:
            self.lastw[b] = tok
            self.readers[b] = {}

    def op(self, eng, fn, reads=(), writes=()):
        self._deps(eng, reads, writes)
        ins = fn(self.eng[eng])
        self.cnt[eng] += 1
        ins.then_inc(self.sem[eng], 1)
        self._commit((eng, self.cnt[eng]), reads, writes)
        self.nops += 1

    def dma(self, q, out, in_, reads=(), writes=()):
        lanes = self.lanes[q]
        i = self.rr[q]
        self.rr[q] = (i + 1) % len(lanes)
        lane = lanes[i]
        key = ("lane", q, i)
        if lane["cnt"] > 0:
            self._wait(q, key, 16 * lane["cnt"])
        self._deps(q, reads, writes)
        ins = self.eng[q].dma_start(out=out, in_=in_)
        lane["cnt"] += 1
        ins.then_inc(lane["sem"], 16)
        self._commit((key, 16 * lane["cnt"]), reads, writes)
        self.nops += 1

    def barrier(self, engines=("pe", "act", "dve", "pool", "sp")):
        for e in engines:
            for k in self.sem:
                if k != e and self.cnt[k] > 0:
                    self._wait(e, k, self.cnt[k])
            for q in self.lanes:
                for i, lane in enumerate(self.lanes[q]):
                    if lane["cnt"] > 0:
                        self._wait(e, ("lane", q, i), 16 * lane["cnt"])

    def finish(self):
        self.barrier(engines=("sp",))


class Ctx:
    pass


_UNIQ = [0]


def _sbt(nc, name, shape, dt):
    _UNIQ[0] += 1
    return nc.sbuf_tensor("%s_u%d" % (name, _UNIQ[0]), shape, dt)


def _mk_psum(nc, es, S, n=8):
    banks = [es.enter_context(nc.psum_tensor("psb%d" % i, [128, 512], F32)) for i in range(n)]
    st = {"i": 0}

    def nxt(lo=0, hi=n):
        i = st["i"]
        if i < lo or i >= hi:
            i = lo
        st["i"] = i + 1 if i + 1 < hi else lo
        return banks[i], ("ps", i)

    return banks, nxt


def emit_norm_mod(S, C, xT, hT, acol, scol, ntok=TOK):
    nt = ntok // 512
    for tt in range(nt):
        ts = slice(tt * 512, (tt + 1) * 512)
        pn, pk = C.nxt()
        for k in range(8):
            sq = C.sq[k % 2]
            S.op("act", lambda e, sq=sq, k=k: e.activation(out=sq[:], in_=xT[:, k, ts], func=AF.Square),
                 reads=[("xT", k, tt)], writes=[("sq", k % 2)])
            S.op("pe", lambda e, sq=sq, k=k: e.matmul(pn[:], C.ones_ms[:], sq[:], start=(k == 0), stop=(k == 7)),
                 reads=[("sq", k % 2)], writes=[pk])
        rs = C.rs[tt % 2]
        S.op("act", lambda e: e.activation(out=rs[:], in_=pn[:], func=AF.Sqrt, bias=C.eps_col[:], scale=1.0),
             reads=[], writes=[pk, ("rs", tt % 2)])
        S.op("dve", lambda e: e.reciprocal(rs[:], rs[:]), reads=[], writes=[("rs", tt % 2)])
        for k in range(8):
            tmp = C.tmp[k % 2]
            S.op("dve", lambda e, tmp=tmp, k=k: e.tensor_tensor(tmp[:], xT[:, k, ts], rs[:], ALU.mult),
                 reads=[("xT", k, tt), ("rs", tt % 2)], writes=[("tmp", k % 2)])
            S.op("act", lambda e, tmp=tmp, k=k: e.activation(out=hT[:, k, ts], in_=tmp[:], func=AF.Identity,
                                                             bias=scol(k), scale=acol(k)),
                 reads=[("tmp", k % 2), "modT"], writes=[("hT", k, tt)])


def emit_ffn(S, C, nc, xT, hT, acol, scol, gcol, w_in, w_out, tag):
    emit_norm_mod(S, C, xT, hT, acol, scol)
    w_in_v = w_in.rearrange("(k p) c -> p k c", p=128)
    w_out_v = w_out.rearrange("(j p) c -> p j c", p=128)
    with contextlib.ExitStack() as es:
        gT = es.enter_context(_sbt(nc, "gT" + tag, [128, 11, TOK], BF16))
        win = [es.enter_context(_sbt(nc, "win%d%s" % (i, tag), [128, 8, 512], BF16)) for i in range(2)]
        wout = [es.enter_context(_sbt(nc, "wout%d%s" % (i, tag), [128, 11, 256], BF16)) for i in range(2)]
        sa = [es.enter_context(_sbt(nc, "sa%d%s" % (i, tag), [128, 512], F32)) for i in range(2)]
        wi_i = 0
        wo_i = 0
        sa_i = 0
        for half in range(2):
            pieces = [(0, 2), (2, 2), (4, 2), (6, 2), (8, 2), (10, 1)]
            for (j0, nj) in pieces:
                wb = win[wi_i % 2]
                wkey = ("win", wi_i % 2)
                wi_i += 1
                ca = (half * 11 + j0) * 128
                ncol = nj * 128
                S.dma("pool", wb[:, :, 0:ncol], w_in_v[:, :, ca:ca + ncol], writes=[wkey])
                S.dma("pool", wb[:, :, 256:256 + ncol], w_in_v[:, :, DFF + ca:DFF + ca + ncol], writes=[wkey])
                for jj in range(nj):
                    jl = j0 + jj
                    for tt in range(4):
                        ts = slice(tt * 512, (tt + 1) * 512)
                        pa, pak = C.nxt()
                        pb, pbk = C.nxt()
                        for k in range(8):
                            S.op("pe", lambda e, k=k: e.matmul(pa[:], wb[:, k, jj * 128:(jj + 1) * 128], hT[:, k, ts],
                                                               start=(k == 0), stop=(k == 7)),
                                 reads=[wkey, ("hT", k, tt)], writes=[pak])
                        for k in range(8):
                            S.op("pe", lambda e, k=k: e.matmul(pb[:], wb[:, k, 256 + jj * 128:256 + (jj + 1) * 128],
                                                               hT[:, k, ts], start=(k == 0), stop=(k == 7)),
                                 reads=[wkey, ("hT", k, tt)], writes=[pbk])
                        sb_ = sa[sa_i % 2]
                        sk = ("sa", sa_i % 2)
                        sa_i += 1
                        S.op("act", lambda e: e.activation(out=sb_[:], in_=pa[:], func=AF.Silu), writes=[pak, sk])
                        S.op("dve", lambda e: e.tensor_tensor(gT[:, jl, ts], sb_[:], pb[:], ALU.mult),
                             reads=[sk], writes=[pbk, ("gT", jl, tt)])
            for o in range(8):
                if o % 2 == 0:
                    wo = wout[wo_i % 2]
                    wok = ("wout", wo_i % 2)
                    wo_i += 1
                    S.dma("pool", wo[:], w_out_v[:, half * 11:(half + 1) * 11, o * 128:(o + 2) * 128], writes=[wok])
                oc = (o % 2) * 128
                for tt in range(4):
                    ts = slice(tt * 512, (tt + 1) * 512)
                    py, pyk = C.nxt()
                    for j in range(11):
                        S.op("pe", lambda e, j=j: e.matmul(py[:], wo[:, j, oc:oc + 128], gT[:, j, ts], start=(j == 0), stop=(j == 10)),
                             reads=[wok, ("gT", j, tt)], writes=[pyk])
                    S.op("dve", lambda e: e.scalar_tensor_tensor(out=xT[:, o, ts], in0=py[:], scalar=gcol(o), in1=xT[:, o, ts],
                                                                 op0=ALU.mult, op1=ALU.add),
                         reads=["modT"], writes=[pyk, ("xT", o, tt)])
        S.barrier()


def emit_consts_common(S, C, nc, es, dram):
    C.ones_ms = es.enter_context(_sbt(nc, "ones_ms", [128, 128], BF16))
    C.eps_col = es.enter_context(_sbt(nc, "eps_col", [128, 1], F32))
    C.sq = [es.enter_context(_sbt(nc, "sq%d" % i, [128, 512], BF16)) for i in range(2)]
    C.rs = [es.enter_context(_sbt(nc, "rs%d" % i, [128, 512], F32)) for i in range(2)]
    C.tmp = [es.enter_context(_sbt(nc, "tmp%d" % i, [128, 512], F32)) for i in range(2)]
    S.dma("sp", C.ones_ms[:], dram["c_ones"][:, :], writes=["ones_ms"])
    S.op("dve", lambda e: e.memset(C.eps_col[:], EPS), writes=["eps_col"])
    C.one_col = es.enter_context(_sbt(nc, "one_col", [128, 1], F32))
    S.op("dve", lambda e: e.memset(C.one_col[:], 1.0), writes=["one_col"])
    S.barrier()


def emit_mod_cols(S, C, nc, es, pv, modT):
    if not hasattr(C, "acols"):
        C.acols = es.enter_context(_sbt(nc, "acols", [128, 24], F32))
        C.hg = es.enter_context(_sbt(nc, "hg", [128, 16], F32))
    for i, (sc0, n0) in enumerate([(8, PV_N1), (32, PV_N2), (56, PV_N3)]):
        S.op("dve", lambda e, i=i, sc0=sc0, n0=n0: e.scalar_tensor_tensor(
            out=C.acols[:, i * 8:(i + 1) * 8], in0=modT[:, sc0:sc0 + 8], scalar=1.0, in1=pv[:, n0:n0 + 8],
            op0=ALU.add, op1=ALU.mult), reads=["modT", "pv"], writes=["acols"])
    S.op("dve", lambda e: e.tensor_scalar(C.hg[:, 0:8], modT[:, 16:24], 0.5, None, ALU.mult), reads=["modT"], writes=["hg"])
    S.op("dve", lambda e: e.tensor_scalar(C.hg[:, 8:16], modT[:, 64:72], 0.5, None, ALU.mult), reads=["modT"], writes=["hg"])
    S.barrier()


def emit_adaln(S, C, nc, pv, modT, ada_w):
    aw = ada_w.rearrange("(k p) c -> p k c", p=128)
    with contextlib.ExitStack() as es:
        cact = es.enter_context(_sbt(nc, "cact", [128, 8], BF16))
        awb = [es.enter_context(_sbt(nc, "awb%d" % i, [128, 8, 1024], BF16)) for i in range(2)]
        S.op("act", lambda e: e.activation(out=cact[:], in_=pv[:, PV_C:PV_C + 8], func=AF.Silu), reads=["pv"], writes=["cact"])
        pm, pmk = C.nxt()
        for g in range(9):
            wb = awb[g % 2]
            wk = ("awb", g % 2)
            for hh in range(2):
                S.dma("pool", wb[:, :, hh * 512:(hh + 1) * 512], aw[:, :, g * 1024 + hh * 512:g * 1024 + (hh + 1) * 512], writes=[wk])
            for jj in range(8):
                j = g * 8 + jj
                for k in range(8):
                    S.op("pe", lambda e, k=k, jj=jj, j=j: e.matmul(pm[:, j:j + 1], wb[:, k, jj * 128:(jj + 1) * 128], cact[:, k:k + 1],
                                                                 start=(k == 0), stop=(k == 7)),
                         reads=[wk, "cact"], writes=[pmk])
        S.op("dve", lambda e: e.tensor_tensor(modT[:, 0:72], pm[:, 0:72], pv[:, PV_AB:PV_AB + 72], ALU.add),
             reads=["pv"], writes=[pmk, "modT"])
        S.barrier()


def emit_rope_tables(S, C, nc, es, pv, pos_ap, ntok=TOK):
    C.cosT = es.enter_context(_sbt(nc, "cosT", [128, ntok], F32))
    C.sinT = es.enter_context(_sbt(nc, "sinT", [128, ntok], F32))
    with contextlib.ExitStack() as es2:
        pi_ = es2.enter_context(_sbt(nc, "pos_i", [128, ntok], I32))
        u = es2.enter_context(_sbt(nc, "rope_u", [128, ntok], F32))
        ni = es2.enter_context(_sbt(nc, "rope_ni", [128, ntok], I32))
        nf = es2.enter_context(_sbt(nc, "rope_nf", [128, ntok], F32))
        f2 = es2.enter_context(_sbt(nc, "rope_f2", [128, ntok], F32))
        S.dma("sp", pi_[:], pos_ap.broadcast_to([128, ntok]), writes=["pos_i"])
        S.op("dve", lambda e: e.tensor_copy(u[:], pi_[:]), reads=["pos_i"], writes=["u"])
        S.op("dve", lambda e: e.tensor_scalar(u[:], u[:], pv[:, PV_IF:PV_IF + 1], None, ALU.mult), reads=["pv"], writes=["u"])
        S.op("dve", lambda e: e.tensor_copy(ni[:], u[:]), reads=["u"], writes=["ni"])
        S.op("dve", lambda e: e.tensor_copy(nf[:], ni[:]), reads=["ni"], writes=["nf"])
        S.op("dve", lambda e: e.tensor_tensor(u[:], u[:], nf[:], ALU.subtract), reads=["nf"], writes=["u"])
        S.op("act", lambda e: e.activation(out=C.sinT[:], in_=u[:], func=AF.Sin, scale=2.0 * np.pi), reads=["u"], writes=["sinT"])
        S.op("dve", lambda e: e.tensor_scalar(f2[:], u[:], 0.25, None, ALU.add), reads=["u"], writes=["f2"])
        S.op("dve", lambda e: e.tensor_scalar(nf[:], f2[:], 0.5, None, ALU.is_gt), reads=["f2"], writes=["nf"])
        S.op("dve", lambda e: e.tensor_tensor(f2[:], f2[:], nf[:], ALU.subtract), reads=["nf"], writes=["f2"])
        S.op("act", lambda e: e.activation(out=C.cosT[:], in_=f2[:], func=AF.Sin, scale=2.0 * np.pi), reads=["f2"], writes=["cosT"])
        S.barrier()


def emit_proj(S, C, nc, xT, hT, pv, wmi, dram, OA, gwb_ap, pos_ap):
    wv = wmi.rearrange("(k p) c -> p k c", p=128)
    with contextlib.ExitStack() as es:
        def sb(name, shape, dt=F32):
            return es.enter_context(_sbt(nc, name, shape, dt))
        W0 = 1024
        w = sb("wmi_sb", [128, 8, INW - W0], BF16)
        tri = sb("tri_f", [128, 128], F32)
        gwb = sb("gwb_sb", [17, 256], F32)
        cgT = sb("cgT", [32, 512], F32)
        e_sb = sb("e_sb", [128, 256], F32)
        sp_sb = sb("sp_sb", [128, 4, 256], F32)
        enG = sb("enG", [128, 4, 256], F32)
        eGT = sb("eGT", [128, 2, 512], F32)
        enGT = sb("enGT", [128, 2, 512], F32)
        ge_sb = sb("ge_sb", [128, 2, 32], F32)
        st_bf = [sb("st_bf%d" % i, [128, 512], BF16) for i in range(3)]
        st_f = [sb("st_f%d" % i, [128, 512], F32) for i in range(2)]
        sg = sb("sg", [128, 512], F32)
        gkst = sb("gkst", [128, 4, 256], BF16)
        gvst = sb("gvst", [128, 4, 256], BF16)
        vst = sb("vst", [128, 4, 512], BF16)
        cnt = {"bf": 0, "f": 0, "x": 0}

        def nbf():
            i = cnt["bf"] % 3
            cnt["bf"] += 1
            return st_bf[i], ("st_bf", i)

        def nf_():
            i = cnt["f"] % 2
            cnt["f"] += 1
            return st_f[i], ("st_f", i)

        for (c0, c1) in [(1024, 2048), (2048, 3088)]:
            S.dma("pool", w[:, :, c0 - W0:c1 - W0], wv[:, :, c0:c1], writes=["wmi"])
        S.dma("sp", tri[:], dram["c_tri"][:, :], writes=["tri"])
        S.dma("sp", gwb[:], gwb_ap, writes=["gwb"])
        S.op("dve", lambda e: e.memset(cgT[:], 1.0), writes=["cgT"])

        def mm8(ps, psk, c0, ncol, ts, tt):
            for k in range(8):
                S.op("pe", lambda e, k=k: e.matmul(ps[0:ncol, :], w[:, k, c0 - W0:c0 - W0 + ncol], hT[:, k, ts], start=(k == 0), stop=(k == 7)),
                     reads=["wmi", ("hT", k, tt)], writes=[psk])

        for tt in range(4):
            ts = slice(tt * 512, (tt + 1) * 512)
            p, pk = C.nxt()
            mm8(p, pk, 3072, 16, ts, tt)
            S.op("act", lambda e: e.copy(cgT[0:16, :], p[0:16, :]), writes=[pk, "cgT"])
            for s in range(4):
                ss = slice(s * 128, (s + 1) * 128)
                pz, pzk = C.nxt()
                S.op("pe", lambda e: e.matmul(pz[:, 0:256], cgT[0:17, ss], gwb[0:17, :], start=True, stop=True),
                     reads=["cgT", "gwb"], writes=[pzk])
                S.op("act", lambda e: e.activation(out=e_sb[:], in_=pz[:, 0:256], func=AF.Exp, scale=-1.0), writes=[pzk, "e_sb"])
                S.op("act", lambda e: e.activation(out=sp_sb[:, s, :], in_=e_sb[:], func=AF.Ln, bias=C.one_col[:], scale=1.0),
                     reads=["e_sb"], writes=[("sp", s)])
                pG, pGk = C.nxt()
                S.op("pe", lambda e: e.matmul(pG[:, 0:256], tri[:], sp_sb[:, s, :], start=True, stop=True),
                     reads=["tri", ("sp", s)], writes=[pGk])
                S.op("act", lambda e: e.activation(out=enG[:, s, :], in_=pG[:, 0:256], func=AF.Exp, scale=-1.0),
                     writes=[pGk, ("enG", s)])
                for hp in range(2):
                    pT_, pTk = C.nxt()
                    S.op("pe", lambda e, hp=hp: e.matmul(pT_[:, 0:128], sp_sb[:, s, hp * 128:(hp + 1) * 128], tri[:], start=True, stop=True),
                         reads=["tri", ("sp", s)], writes=[pTk])
                    S.op("act", lambda e, hp=hp: e.activation(out=eGT[:, hp, ss], in_=pT_[:, 0:128], func=AF.Exp),
                         writes=[pTk, ("eGT", hp)])
                    S.op("act", lambda e, hp=hp: e.activation(out=enGT[:, hp, ss], in_=pT_[:, 0:128], func=AF.Exp, scale=-1.0),
                         writes=[pTk, ("enGT", hp)])
            for hp in range(2):
                S.op("act", lambda e, hp=hp: e.copy(ge_sb[:, hp, tt * 8:(tt + 1) * 8], eGT[:, hp, 63:512:64]),
                     reads=[("eGT", hp)], writes=["ge_sb"])
            for hp in range(2):
                p, pk = C.nxt()
                mm8(p, pk, 2048 + hp * 128, 128, ts, tt)
                st, sk = nbf()
                S.op("dve", lambda e, hp=hp: e.scalar_tensor_tensor(out=st[:], in0=p[:], scalar=0.125, in1=eGT[:, hp, :],
                                                                     op0=ALU.mult, op1=ALU.mult),
                     reads=[("eGT", hp)], writes=[pk, sk])
                S.dma("sp", OA.fm("GQ", hp, ts), st[:], reads=[sk])
                p, pk = C.nxt()
                mm8(p, pk, 2304 + hp * 128, 128, ts, tt)
                st, sk = nbf()
                S.op("dve", lambda e, hp=hp: e.tensor_tensor(st[:], p[:], enGT[:, hp, :], ALU.mult),
                     reads=[("enGT", hp)], writes=[pk, sk])
                S.dma("sp", OA.fm("GK", hp, ts), st[:], reads=[sk])
                p, pk = C.nxt()
                mm8(p, pk, 2816 + hp * 128, 128, ts, tt)
                st, sk = nbf()
                S.op("act", lambda e: e.activation(out=st[:], in_=p[:], func=AF.Silu), writes=[pk, sk])
                S.dma("sp", OA.fm("GR", hp, ts), st[:], reads=[sk])
            for s in range(4):
                tok = slice(tt * 512 + s * 128, tt * 512 + (s + 1) * 128)
                p, pk = C.nxt()
                for k in range(8):
                    S.op("pe", lambda e, k=k: e.matmul(p[:], hT[:, k, tok], w[:, k, 2304 - W0:2816 - W0], start=(k == 0), stop=(k == 7)),
                         reads=["wmi", ("hT", k, tt)], writes=[pk])
                S.op("dve", lambda e: e.tensor_tensor(gkst[:, s, :], p[:, 0:256], enG[:, s, :], ALU.mult),
                     reads=[("enG", s)], writes=[pk, "gkst"])
                S.op("act", lambda e: e.copy(gvst[:, s, :], p[:, 256:512]), writes=[pk, "gvst"])
            for hs in range(2):
                for s4 in range(4):
                    S.dma("sp", OA.tm2("GKT", tt, s4, hs), gkst[:, s4, hs * 128:(hs + 1) * 128], reads=["gkst"])
                    S.dma("sp", OA.tm2("GV", tt, s4, hs), gvst[:, s4, hs * 128:(hs + 1) * 128], reads=["gvst"])
            for s in range(4):
                tok = slice(tt * 512 + s * 128, tt * 512 + (s + 1) * 128)
                p, pk = C.nxt()
                for k in range(8):
                    S.op("pe", lambda e, k=k: e.matmul(p[:], hT[:, k, tok], w[:, k, 1024 - W0:1536 - W0], start=(k == 0), stop=(k == 7)),
                         reads=["wmi", ("hT", k, tt)], writes=[pk])
                S.op("act", lambda e: e.copy(vst[:, s, :], p[:]), writes=[pk, "vst"])
            for hs in range(2):
                for s4 in range(4):
                    S.dma("sp", OA.tm2("V", tt, s4, hs), vst[:, s4, hs * 256:(hs + 1) * 256], reads=["vst"])
            for cc in range(2):
                pa, pak = C.nxt()
                mm8(pa, pak, 1536 + cc * 128, 128, ts, tt)
                pg, pgk = C.nxt()
                mm8(pg, pgk, 1792 + cc * 128, 128, ts, tt)
                S.op("act", lambda e: e.activation(out=sg[:], in_=pg[:], func=AF.Sigmoid), writes=[pgk, "sg"])
                st, sk = nf_()
                S.op("dve", lambda e: e.tensor_tensor(st[:], pa[:], sg[:], ALU.mult), reads=["sg"], writes=[pak, sk])
                S.dma("sp", OA.fm("HC", cc, ts), st[:], reads=[sk])
        for hp in range(2):
            S.dma("sp", OA.fm("GE", hp, slice(0, 32)), ge_sb[:, hp, :], reads=["ge_sb"])
        S.barrier()
    with contextlib.ExitStack() as es:
        def sb(name, shape, dt=F32):
            return es.enter_context(_sbt(nc, name, shape, dt))
        emit_rope_tables(S, C, nc, es, pv, pos_ap)
        w = sb("wqk_sb", [128, 8, 1024], BF16)
        W0 = 0
        blk = sb("blk_bf", [128, 128], BF16)
        rot = sb("rot_f", [128, 128], F32)
        km_sb = sb("km_sb", [128, 4, 8], F32)
        st_bf = [sb("st2_bf%d" % i, [128, 512], BF16) for i in range(3)]
        tn = [sb("tn%d" % i, [128, 512], F32) for i in range(3)]
        uu = [sb("uu%d" % i, [128, 512], F32) for i in range(3)]
        ww = [sb("ww%d" % i, [128, 512], F32) for i in range(3)]
        rsq = [sb("rsq%d" % i, [128, 512], F32) for i in range(3)]
        sqb = [sb("sqb%d" % i, [128, 512], BF16) for i in range(3)]
        S.dma("pool", w[:, :, :], wv[:, :, 0:1024], writes=["wmi"])
        S.dma("sp", blk[:], dram["c_blk"][:, :], writes=["blk"])
        S.dma("sp", rot[:], dram["c_rot"][:, :], writes=["rot"])
        cnt = {"bf": 0, "x": 0}

        def nbf():
            i = cnt["bf"] % 3
            cnt["bf"] += 1
            return st_bf[i], ("st_bf", i)

        def mm8(ps, psk, c0, ncol, ts, tt):
            for k in range(8):
                S.op("pe", lambda e, k=k: e.matmul(ps[0:ncol, :], w[:, k, c0:c0 + ncol], hT[:, k, ts], start=(k == 0), stop=(k == 7)),
                     reads=["wmi", ("hT", k, tt)], writes=[psk])

        its = [(tt, which, hp) for tt in range(4) for which in range(2) for hp in range(4)]
        st8 = {}

        def stage1(n):
            tt, which, hp = its[n]
            ts = slice(tt * 512, (tt + 1) * 512)
            i = n % 3
            p, pk = C.nxt()
            mm8(p, pk, which * 512 + hp * 128, 128, ts, tt)
            S.op("act", lambda e: e.activation(out=sqb[i][:], in_=p[:], func=AF.Square), writes=[pk, ("sqb", i)])
            st8[n] = (p, pk)

        def stage2(n):
            tt, which, hp = its[n]
            i = n % 3
            p, pk = st8.pop(n)
            gain_col = pv[:, PV_QN + which:PV_QN + which + 1]
            pm, pmk = C.nxt()
            S.op("pe", lambda e: e.matmul(pm[:], blk[:], sqb[i][:], start=True, stop=True), reads=["blk", ("sqb", i)], writes=[pmk])
            S.op("act", lambda e: e.activation(out=rsq[i][:], in_=pm[:], func=AF.Sqrt, bias=C.eps_col[:], scale=1.0),
                 writes=[pmk, ("rsq", i)])
            S.op("dve", lambda e: e.reciprocal(rsq[i][:], rsq[i][:]), writes=[("rsq", i)])
            S.op("dve", lambda e: e.scalar_tensor_tensor(out=tn[i][:], in0=p[:], scalar=gain_col, in1=rsq[i][:],
                                                         op0=ALU.mult, op1=ALU.mult),
                 reads=["pv", ("rsq", i)], writes=[pk, ("tn", i)])

        def stage3(n):
            tt, which, hp = its[n]
            ts = slice(tt * 512, (tt + 1) * 512)
            i = n % 3
            pr, prk = C.nxt()
            S.op("pe", lambda e: e.matmul(pr[:], rot[:], tn[i][:], start=True, stop=True), reads=["rot", ("tn", i)], writes=[prk])
            S.op("pool", lambda e: e.tensor_tensor(uu[i][:], tn[i][:], C.cosT[:, ts], ALU.mult),
                 reads=[("tn", i), "cosT"], writes=[("uu", i)])
            S.op("dve", lambda e: e.tensor_tensor(ww[i][:], pr[:], C.sinT[:, ts], ALU.mult), reads=["sinT"], writes=[prk, ("ww", i)])
            S.op("dve", lambda e: e.tensor_tensor(uu[i][:], uu[i][:], ww[i][:], ALU.add), reads=[("ww", i)], writes=[("uu", i)])
            st, sk = nbf()
            S.op("act", lambda e: e.copy(st[:], uu[i][:]), reads=[("uu", i)], writes=[sk])
            name = "QT" if which == 0 else "KT"
            S.dma("sp", OA.fm(name, hp, ts), st[:], reads=[sk])
            if which == 1:
                S.op("dve", lambda e: e.tensor_reduce(out=km_sb[:, hp, tt * 2:(tt + 1) * 2],
                                                      in_=uu[i][:].rearrange("p (b t) -> p b t", b=2), axis=AX.X, op=ALU.add),
                     reads=[("uu", i)], writes=["km_sb"])

        NI = len(its)
        for n in range(NI + 2):
            if n < NI:
                stage1(n)
            if 0 <= n - 1 < NI:
                stage2(n - 1)
            if 0 <= n - 2 < NI:
                stage3(n - 2)
        for hp in range(4):
            S.dma("sp", OA.fm("KM", hp, slice(0, 8)), km_sb[:, hp, :], reads=["km_sb"])
        S.barrier()


def emit_conv(S, C, nc, pvB, din, IB, OB):
    with contextlib.ExitStack() as es:
        def sb(name, shape, dt=F32):
            return es.enter_context(_sbt(nc, name, shape, dt))
        hc = sb("hc", [128, 30 + SEQ], F32)
        acc = [sb("cacc%d" % i, [128, 512], F32) for i in range(2)]
        xc = [sb("cxc%d" % i, [128, 512], F32) for i in range(2)]
        sq = [sb("csq%d" % i, [128, 512], F32) for i in range(2)]
        sd = [sb("csd%d" % i, [128, 512], F32) for i in range(2)]
        ob = [sb("cob%d" % i, [128, 512], BF16) for i in range(2)]
        blkf = sb("blkf", [128, 128], F32)
        S.dma("sp", blkf[:], din["c_blkf"][:, :], writes=["blkf"])
        S.op("dve", lambda e: e.memset(hc[:, 0:30], 0.0), writes=[("hc", -1)])
        for tt in range(8):
            S.dma("sp", hc[:, 30 + tt * 512:30 + (tt + 1) * 512], IB.fm("sp", "HC", 0, 128, tt // 4, slice((tt % 4) * 512, (tt % 4 + 1) * 512)), writes=[("hc", tt)])
        yield
        for tt in range(8):
            i = tt % 2
            hk = [("hc", tt), ("hc", tt - 1)]
            a = acc[i]
            S.op("dve", lambda e: e.tensor_scalar(a[:], hc[:, tt * 512:tt * 512 + 512], pvB[:, PB_CW:PB_CW + 1], pvB[:, PB_CB:PB_CB + 1],
                                                  ALU.mult, ALU.add), reads=hk + ["pvB"], writes=[("cacc", i)])
            for j in range(1, 31):
                S.op("dve", lambda e, j=j: e.scalar_tensor_tensor(out=a[:], in0=hc[:, tt * 512 + j:tt * 512 + j + 512],
                                                                  scalar=pvB[:, PB_CW + j:PB_CW + j + 1], in1=a[:],
                                                                  op0=ALU.mult, op1=ALU.add),
                     reads=hk + ["pvB"], writes=[("cacc", i)])
                if j % 4 == 0:
                    yield
            pm, pmk = C.nxt(0, 6)
            S.op("pe", lambda e: e.matmul(pm[:], blkf[:], a[:], start=True, stop=True), reads=["blkf", ("cacc", i)], writes=[pmk])
            S.op("dve", lambda e: e.tensor_tensor(xc[i][:], a[:], pm[:], ALU.subtract), reads=[("cacc", i)], writes=[pmk, ("cxc", i)])
            S.op("act", lambda e: e.activation(out=sq[i][:], in_=xc[i][:], func=AF.Square), reads=[("cxc", i)], writes=[("csq", i)])
            pv_, pvk = C.nxt(0, 6)
            S.op("pe", lambda e: e.matmul(pv_[:], blkf[:], sq[i][:], start=True, stop=True), reads=["blkf", ("csq", i)], writes=[pvk])
            S.op("act", lambda e: e.activation(out=sd[i][:], in_=pv_[:], func=AF.Sqrt, bias=C.eps_col[:], scale=1.0),
                 writes=[pvk, ("csd", i)])
            S.op("dve", lambda e: e.reciprocal(sd[i][:], sd[i][:]), writes=[("csd", i)])
            S.op("dve", lambda e: e.tensor_tensor(xc[i][:], xc[i][:], sd[i][:], ALU.mult), reads=[("csd", i)], writes=[("cxc", i)])
            S.op("act", lambda e: e.activation(out=ob[i][:], in_=xc[i][:], func=AF.Silu, bias=pvB[:, PB_CBE:PB_CBE + 1],
                                               scale=pvB[:, PB_CG:PB_CG + 1]), reads=[("cxc", i), "pvB"], writes=[("cob", i)])
            S.dma("sp", OB(256, 128, tt * 512, 512), ob[i][:], reads=[("cob", i)])
            yield
        while not C.conv_release:
            yield
        S.barrier()


def emit_gla(S, C, nc, pvB, din, IB, OB):
    NT = SEQ // 128
    with contextlib.ExitStack() as es:
        def sb(name, shape, dt=F32):
            return es.enter_context(_sbt(nc, name, shape, dt))
        qh = [sb("gq%d" % h, [128, SEQ], BF16) for h in range(2)]
        ktT = sb("gktT", [128, SEQ], BF16)
        ktc = [sb("gktc%d" % c, [128, NT, 128], BF16) for c in range(2)]
        vpad = sb("gvpad", [128, NT, 2, 128], BF16)
        AT = sb("gAT", [128, NT, 2, 128], BF16)
        Sall = sb("gSall", [128, 65, 128], F32)
        Sb = sb("gSb", [128, 64, 128], BF16)
        ge = sb("gge", [128, 64], F32)
        gmask = sb("ggmask", [128, 128], BF16)
        bmask = sb("gbmask", [128, 128], F32)
        blkf = sb("gblkf", [128, 128], F32)
        og = [C.tmp[0]] * 2
        gsq = [C.tmp[1]] * 2
        grs = [C.rs[0]] * 2
        gr = [sb("ggr%d" % i, [128, 512], BF16) for i in range(2)]
        gout = [sb("gout%d" % i, [128, 512], BF16) for i in range(2)]
        for t_, nm in ((gmask, "c_gmask"), (bmask, "c_bmask"), (blkf, "c_blkf")):
            S.dma("sp", t_[:], din[nm][:, :], writes=[nm])
        for th in range(2):
            S.dma("sp", ge[:, th * 32:(th + 1) * 32], IB.fm("sp", "GE", 0, 128, th, slice(0, 32)), writes=["gge"])
        S.op("dve", lambda e: e.memset(qh[0][64:128, :], 0.0), writes=["gq0z"])
        S.op("dve", lambda e: e.memset(qh[1][0:64, :], 0.0), writes=["gq1z"])
        S.op("pool", lambda e: e.memset(ktc[0][64:128, :, :], 0.0), writes=["gktc0z"])
        S.op("pool", lambda e: e.memset(ktc[1][0:64, :, :], 0.0), writes=["gktc1z"])
        S.op("pool", lambda e: e.memset(vpad[:], 0.0), writes=["gvpad"])
        S.op("dve", lambda e: e.memset(Sall[:, 0, :], 0.0), writes=[("S", 0)])
        HT_ = NT // 2
        for th in range(2):
            tk = slice(th * TOK, (th + 1) * TOK)
            tl = slice(th * HT_, (th + 1) * HT_)
            S.dma("sp", qh[0][0:64, tk], IB.fm("sp", "GQ", 0, 64, th, slice(0, TOK)), writes=["gq0"])
            S.dma("sp", qh[1][64:128, tk], IB.fm("sp", "GQ", 64, 64, th, slice(0, TOK)), writes=["gq1"])
            S.dma("sp", ktT[:, tk], IB.fm("sp", "GK", 0, 128, th, slice(0, TOK)), writes=["gktT"])
            gkt_v = IB.tm("sp", "GKT", th).rearrange("(t c p) f -> c p t f", c=2, p=64)
            S.dma("sp", ktc[0][0:64, tl, :], gkt_v[0], writes=["gktc0"])
            S.dma("sp", ktc[1][64:128, tl, :], gkt_v[1], writes=["gktc1"])
            gv_v = IB.tm("sp", "GV", th).rearrange("(t p) f -> p t f", p=128)
            for h in range(2):
                S.dma("sp", vpad[:, tl, h, h * 64:(h + 1) * 64], gv_v[:, :, h * 64:(h + 1) * 64], reads=[], writes=["gvpad"])
        for t in range(NT):
            tsl = slice(t * 128, (t + 1) * 128)
            for c in range(2):
                n = 2 * t + c
                p, pk = C.nxt()
                for h2 in range(2):
                    S.op("pe", lambda e, h2=h2: e.matmul(p[:, 0:128], ktc[c][:, t, :], vpad[:, t, h2, :], start=(h2 == 0), stop=(h2 == 1)),
                         reads=["gktc%d" % c, "gktc%dz" % c, "gvpad"], writes=[pk])
                S.op("dve", lambda e: e.scalar_tensor_tensor(out=Sall[:, n + 1, :], in0=p[:, 0:128], scalar=ge[:, n:n + 1], in1=bmask[:],
                                                             op0=ALU.mult, op1=ALU.mult),
                     reads=["gge", "c_bmask"], writes=[pk, ("S", n + 1)])
            for h in range(2):
                p, pk = C.nxt()
                S.op("pe", lambda e: e.matmul(p[:, 0:128], ktT[:, tsl], qh[h][:, tsl], start=True, stop=True),
                     reads=["gktT", "gq%d" % h, "gq%dz" % h], writes=[pk])
                S.op("dve", lambda e: e.tensor_tensor(AT[:, t, h, :], p[:, 0:128], gmask[:], ALU.mult),
                     reads=["c_gmask"], writes=[pk, ("AT", t)])
        for n in range(64):
            S.op("dve", lambda e: e.scalar_tensor_tensor(out=Sall[:, n + 1, :], in0=Sall[:, n, :], scalar=ge[:, n:n + 1], in1=Sall[:, n + 1, :],
                                                         op0=ALU.mult, op1=ALU.add),
                 reads=[("S", n), "gge"], writes=[("S", n + 1)])
            if n % 16 == 15:
                g0 = n - 15
                S.op("act", lambda e: e.copy(Sb[:, g0:g0 + 16, :], Sall[:, g0:g0 + 16, :]),
                     reads=[("S", i) for i in range(g0, g0 + 16)], writes=[("Sb", g0 // 16)])
        for tt in range(8):
            i = tt % 2
            S.dma("sp", gr[i][:], IB.fm("sp", "GR", 0, 128, tt // 4, slice((tt % 4) * 512, (tt % 4 + 1) * 512)), writes=[("ggr", i)])
            po, pok = C.nxt()
            for s4 in range(4):
                t = tt * 4 + s4
                osl = slice(s4 * 128, (s4 + 1) * 128)
                S.op("pe", lambda e: e.matmul(po[:, osl], vpad[:, t, 0, :], AT[:, t, 0, :], start=True, stop=False),
                     reads=["gvpad", ("AT", t)], writes=[pok])
                S.op("pe", lambda e: e.matmul(po[:, osl], vpad[:, t, 1, :], AT[:, t, 1, :], start=False, stop=False),
                     reads=["gvpad", ("AT", t)], writes=[pok])
                for c in range(2):
                    n = 2 * t + c
                    for h2 in range(2):
                        S.op("pe", lambda e, h2=h2: e.matmul(po[:, s4 * 128 + c * 64:s4 * 128 + (c + 1) * 64], Sb[:, n, :],
                                                             qh[h2][:, t * 128 + c * 64:t * 128 + (c + 1) * 64], start=False,
                                                             stop=(c == 1 and h2 == 1)),
                             reads=[("Sb", n // 16), "gq%d" % h2, "gq%dz" % h2], writes=[pok])
            S.op("act", lambda e: e.copy(og[i][:], po[:]), writes=[pok, ("gog", 0)])
            S.op("act", lambda e: e.activation(out=gsq[i][:], in_=og[i][:], func=AF.Square), reads=[("gog", 0)], writes=[("gsq", 0)])
            pm, pmk = C.nxt()
            S.op("pe", lambda e: e.matmul(pm[:], blkf[:], gsq[i][:], start=True, stop=True), reads=["c_blkf", ("gsq", 0)], writes=[pmk])
            S.op("act", lambda e: e.activation(out=grs[i][:], in_=pm[:], func=AF.Sqrt, bias=C.eps_col[:], scale=1.0),
                 writes=[pmk, ("grs", 0)])
            S.op("dve", lambda e: e.reciprocal(grs[i][:], grs[i][:]), writes=[("grs", 0)])
            S.op("dve", lambda e: e.scalar_tensor_tensor(out=og[i][:], in0=og[i][:], scalar=pvB[:, PB_GN:PB_GN + 1], in1=grs[i][:],
                                                         op0=ALU.mult, op1=ALU.mult), reads=["pvB", ("grs", 0)], writes=[("gog", 0)])
            S.op("dve", lambda e: e.tensor_tensor(gout[i][:], og[i][:], gr[i][:], ALU.mult), reads=[("ggr", i), ("gog", 0)],
                 writes=[("gout", i)])
            S.dma("sp", OB(384, 128, tt * 512, 512), gout[i][:], reads=[("gout", i)])
        S.barrier()


def emit_attn(S, C, nc, pvB, din, IB, OB, bg=None):
    NCH = SEQ // 128
    with contextlib.ExitStack() as es:
        def sb(name, shape, dt=F32):
            return es.enter_context(_sbt(nc, name, shape, dt))
        qa = [sb("qa%d" % i, [80, SEQ], BF16) for i in range(2)]
        ka = [sb("ka%d" % i, [80, SEQ], BF16) for i in range(2)]
        va = [sb("va%d" % i, [128, NCH, 128], BF16) for i in range(2)]
        kmf = [sb("kmf%d" % i, [64, 16], F32) for i in range(2)]
        kmb = [sb("kmb%d" % i, [64, 16], BF16) for i in range(2)]
        G1 = sb("G1", [128, 24, 16], F32)
        G2 = sb("G2", [128, 24, 16], F32)
        GE_ = sb("GE_", [128, 24, 16], F32)
        gm = sb("gm", [128, 24], F32)
        PMk = sb("PMk", [128, 24, 16], F32)
        NMk = sb("NMk", [128, 24, 16], F32)
        b80a = sb("b80a", [128, 24, 80], F32)
        pT = [sb("pT%d" % i, [128, 512], BF16) for i in range(3)]
        rden = [sb("rden%d" % i, [128, 256], F32) for i in range(2)]
        ost = [sb("ost%d" % i, [64, 256], BF16) for i in range(2)]
        cmask = sb("cmask", [128, 512], BF16)
        ident = sb("ident", [128, 128], F32)
        S.dma("sp", cmask[:], din["c_cmask"][:, :], writes=["cmask"])
        S.dma("sp", ident[:], din["c_ident"][:, :], writes=["ident"])
        S.dma("sp", PMk[:], din["c_pastmask"].rearrange("p (t j) -> p t j", j=16), writes=["pmk"])
        S.dma("sp", NMk[:], din["c_negmask"].rearrange("p (t j) -> p t j", j=16), writes=["nmk"])
        S.op("dve", lambda e: e.memset(b80a[:], 0.0), writes=["b80a"])
        for i in range(2):
            S.dma("sp", ka[i][64:80, :], din["c_onehot"][:, :], writes=[("ka_oh", i)])
            S.op("dve", lambda e, i=i: e.memset(qa[i][64:80, 0:1024], 0.0), writes=[("qa_z", i)])
            S.op("pool", lambda e, i=i: e.memset(va[i][:], 1.0), writes=[("va", i)])
        cnt = {"pt": 0, "o": 0, "g": 0}
        PO = [6, 7]
        for h in range(4):
            hb = h % 2
            for th in range(2):
                tk = slice(th * TOK, (th + 1) * TOK)
                S.dma("pool", qa[hb][0:64, tk], IB.fm("pool", "QT", h * 64, 64, th, slice(0, TOK)), writes=[("qa", hb)])
                S.dma("pool", ka[hb][0:64, tk], IB.fm("pool", "KT", h * 64, 64, th, slice(0, TOK)), writes=[("ka", hb)])
                vv = IB.tm("pool", "V", th).rearrange("(c p) f -> p c f", p=128)
                for c4 in range(2):
                    S.dma("pool", va[hb][:, th * 16 + c4 * 8:th * 16 + (c4 + 1) * 8, 0:64], vv[:, c4 * 8:(c4 + 1) * 8, h * 64:(h + 1) * 64],
                          writes=[("va", hb)])
                S.dma("pool", kmf[hb][:, th * 8:(th + 1) * 8], IB.fm("pool", "KM", h * 64, 64, th, slice(0, 8)), writes=[("kmf", hb)])
            S.op("act", lambda e: e.mul(kmb[hb][:], kmf[hb][:], 1.0 / 256.0), reads=[("kmf", hb)], writes=[("kmb", hb)])
            pg, pgk = C.nxt(0, 6)
            for t in range(24):
                qs = slice((8 + t) * 128, (9 + t) * 128)
                S.op("pe", lambda e: e.matmul(pg[:, t * 16:(t + 1) * 16], qa[hb][0:64, qs], kmb[hb][0:64, :], start=True, stop=True),
                     reads=[("qa", hb), ("kmb", hb)], writes=[pgk])
            mb = lambda: gm[:].unsqueeze(2).broadcast_to([128, 24, 16])
            S.op("dve", lambda e: e.tensor_tensor(G1[:], pg[:, 0:384].rearrange("p (t j) -> p t j", j=16), PMk[:], ALU.add),
                 reads=["pmk"], writes=[pgk, "G1"])
            S.op("dve", lambda e: e.reduce_max(gm[:], G1[:], AX.X), reads=["G1"], writes=["gm"])
            S.op("dve", lambda e: e.tensor_tensor(GE_[:], G1[:], mb(), ALU.is_ge), reads=["G1", "gm"], writes=["GE"])
            S.op("dve", lambda e: e.scalar_tensor_tensor(out=G2[:], in0=GE_[:], scalar=-1.0e30, in1=G1[:], op0=ALU.mult, op1=ALU.add),
                 reads=["GE", "G1"], writes=["G2"])
            S.op("dve", lambda e: e.reduce_max(gm[:], G2[:], AX.X), reads=["G2"], writes=["gm"])
            S.op("dve", lambda e: e.tensor_tensor(GE_[:], G2[:], mb(), ALU.is_ge), reads=["G2", "gm"], writes=["GE"])
            S.op("dve", lambda e: e.scalar_tensor_tensor(out=G2[:], in0=GE_[:], scalar=-1.0e30, in1=G2[:], op0=ALU.mult, op1=ALU.add),
                 reads=["GE"], writes=["G2"])
            S.op("dve", lambda e: e.reduce_max(gm[:], G2[:], AX.X), reads=["G2"], writes=["gm"])
            S.op("dve", lambda e: e.tensor_tensor(GE_[:], G1[:], mb(), ALU.is_lt), reads=["G1", "gm"], writes=["GE"])
            S.op("dve", lambda e: e.tensor_tensor(b80a[:, :, 64:80], GE_[:], NMk[:], ALU.mult), reads=["GE", "nmk"], writes=["b80a"])
            for t4 in range(6):
                p2, p2k = C.nxt(0, 6)
                for u in range(4):
                    S.op("pe", lambda e: e.matmul(p2[0:80, u * 128:(u + 1) * 128], b80a[:, t4 * 4 + u, :], ident[:], start=True, stop=True),
                         reads=["b80a", "ident"], writes=[p2k])
                q0 = (8 + t4 * 4) * 128
                S.op("act", lambda e: e.copy(qa[hb][64:80, q0:q0 + 512], p2[64:80, 0:512]), writes=[p2k, ("qa", hb)])
            steps = [(i, j) for i in range(16) for j in range(i + 1)]

            def emit_scores(k):
                i, j = steps[k]
                qs = slice(i * 256, (i + 1) * 256)
                ps, psk = C.nxt(0, 6)
                for c in range(2):
                    ks = slice((2 * j + c) * 128, (2 * j + c + 1) * 128)
                    S.op("pe", lambda e: e.matmul(ps[:, c * 256:(c + 1) * 256], ka[hb][0:80, ks], qa[hb][0:80, qs], start=True, stop=True),
                         reads=[("ka", hb), ("ka_oh", hb), ("qa", hb), ("qa_z", hb)], writes=[psk])
                return ps, psk

            pending = emit_scores(0)
            for k, (i, j) in enumerate(steps):
                ps, psk = pending
                if k + 1 < len(steps):
                    pending = emit_scores(k + 1)
                if j == 0:
                    oi = cnt["o"] % 2
                    cnt["o"] += 1
                po = C.banks[PO[oi]]
                pok = ("ps", PO[oi])
                pi = cnt["pt"] % 3
                cnt["pt"] += 1
                S.op("act", lambda e: e.activation(out=pT[pi][:], in_=ps[:], func=AF.Exp, scale=0.125), writes=[psk, ("pT", pi)])
                if j == i:
                    S.op("dve", lambda e: e.tensor_tensor(pT[pi][:], pT[pi][:], cmask[:], ALU.mult), reads=["cmask"], writes=[("pT", pi)])
                for c in range(2):
                    S.op("pe", lambda e: e.matmul(po[:, 0:256], va[hb][:, 2 * j + c, :], pT[pi][:, c * 256:(c + 1) * 256],
                                                  start=(j == 0 and c == 0), stop=(j == i and c == 1)),
                         reads=[("va", hb), ("pT", pi)], writes=[pok])
                if j == i:
                    S.op("dve", lambda e: e.reciprocal(rden[oi][64:128, :], po[64:128, 0:256]), writes=[pok, ("rden", oi)])
                    S.op("dve", lambda e: e.tensor_tensor(ost[oi][:], po[0:64, 0:256], rden[oi][64:128, :], ALU.mult),
                         reads=[("rden", oi)], writes=[pok, ("ost", oi)])
                    S.dma("sp", OB(h * 64, 64, i * 256, 256), ost[oi][:], reads=[("ost", oi)])
                    if bg is not None:
                        for _ in range(1 if i < 8 else 2):
                            next(bg, None)
        S.barrier()


A_OUT_SPECS = {
    "x1T": ([D, TOK], F32), "modT": ([128, 72], F32),
    "QT": ([512, TOK], BF16), "KT": ([512, TOK], BF16), "KM": ([512, 8], F32), "V": ([TOK, 512], BF16),
    "HC": ([256, TOK], F32), "GQ": ([256, TOK], BF16), "GK": ([256, TOK], BF16), "GKT": ([TOK, 256], BF16),
    "GV": ([TOK, 256], BF16), "GR": ([256, TOK], BF16), "GE": ([256, 32], F32),
}
CONST_SPECS = {
    "c_ones": ([128, 128], BF16), "c_blk": ([128, 128], BF16), "c_rot": ([128, 128], F32), "c_tri": ([128, 128], F32),
    "c_onehot": ([16, SEQ], BF16), "c_cmask": ([128, 512], BF16), "c_gmask": ([128, 128], BF16),
    "c_bmask": ([128, 128], F32), "c_blkf": ([128, 128], F32), "c_ident": ([128, 128], F32),
    "c_pastmask": ([128, 384], F32), "c_negmask": ([128, 384], F32),
}


def make_consts():
    c = {}
    c["c_ones"] = np.full((128, 128), 1.0 / 1024.0, np.float32).astype(NPBF)
    blk = np.zeros((128, 128), np.float32)
    blk[:64, :64] = 1.0 / 64.0
    blk[64:, 64:] = 1.0 / 64.0
    c["c_blk"] = blk.astype(NPBF)
    c["c_blkf"] = blk.copy()
    rot = np.zeros((128, 128), np.float32)
    for p in range(128):
        d = p % 64
        if d < 32:
            rot[p + 32, p] = -1.0
        else:
            rot[p - 32, p] = 1.0
    c["c_rot"] = rot
    m = np.arange(128)[:, None]
    l = np.arange(128)[None, :]
    same = (m // 64) == (l // 64)
    c["c_tri"] = np.where(same & (m <= l), -1.0 / 16.0, 0.0).astype(np.float32)
    c["c_gmask"] = np.where(same & (m <= l), 1.0, 0.0).astype(np.float32).astype(NPBF)
    c["c_bmask"] = same.astype(np.float32)
    oh = np.zeros((16, SEQ), np.float32)
    for j in range(16):
        oh[j, j * 256:(j + 1) * 256] = 1.0
    c["c_onehot"] = oh.astype(NPBF)
    kk = np.arange(128)[:, None, None] + 128 * np.arange(2)[None, :, None]
    qq = np.arange(256)[None, None, :]
    c["c_cmask"] = (kk <= qq).astype(np.float32).reshape(128, 512).astype(NPBF)
    c["c_ident"] = np.eye(128, dtype=np.float32)
    pm = np.zeros((128, 24, 16), np.float32)
    nm = np.zeros((128, 24, 16), np.float32)
    for t in range(24):
        i = (8 + t) // 2
        pm[:, t, i:] = -1.0e30
        nm[:, t, :i] = NEGB
    c["c_pastmask"] = pm.reshape(128, 384)
    c["c_negmask"] = nm.reshape(128, 384)
    return c


def _decl_in(nc, dram, name, shape, dt):
    dram[name] = nc.dram_tensor(name, list(shape), dt, kind="ExternalInput").ap()


def _decl_out(nc, dram, name, shape, dt):
    dram[name] = nc.dram_tensor(name, list(shape), dt, kind="ExternalOutput").ap()


def _cols(v):
    return np.ascontiguousarray(np.asarray(v, np.float32).reshape(-1, 128).T)


def make_pv(inp, l, b):
    pv = np.zeros((128, PV_W), np.float32)
    pv[:, PV_AB:PV_AB + 72] = _cols(inp["ada_b"][l])
    pv[:, PV_N1:PV_N1 + 8] = _cols(inp["ffn1_norm"][l])
    pv[:, PV_N2:PV_N2 + 8] = _cols(inp["mix_norm"][l])
    pv[:, PV_N3:PV_N3 + 8] = _cols(inp["ffn2_norm"][l])
    pv[:, PV_C:PV_C + 8] = _cols(inp["c"][b])
    pv[:, PV_QN] = np.tile(np.asarray(inp["q_norm"][l], np.float32), 2)
    pv[:, PV_KN] = np.tile(np.asarray(inp["k_norm"][l], np.float32), 2)
    inv_freq = (1.0 / (np.float32(10000.0) ** (np.arange(0, 64, 2, dtype=np.float32) / np.float32(64)))).astype(np.float32)
    pv[:, PV_IF] = np.tile(inv_freq, 4).astype(np.float64) / (2.0 * np.pi)
    return pv


def emit_outproj(S, C, nc, xT, hT, modT, wmo, IC):
    with contextlib.ExitStack() as es:
        w = es.enter_context(_sbt(nc, "wmo_sb", [128, 8, D], BF16))
        for k in range(8):
            for tt in range(4):
                S.dma("sp", hT[:, k, tt * 512:(tt + 1) * 512], IC(k, slice(tt * 512, (tt + 1) * 512)), writes=[("hT", k, tt)])
        for kc in range(8):
            hh, q = kc // 4, kc % 4
            r0 = (hh * 256 + q * 128) if q < 2 else ((512 + hh * 128) if q == 2 else (768 + hh * 128))
            S.dma("pool", w[:, kc, :], wmo[r0:r0 + 128, :], writes=["wmo"])
        for tt in range(4):
            ts = slice(tt * 512, (tt + 1) * 512)
            for o in range(8):
                py, pyk = C.nxt()
                for kc in range(8):
                    S.op("pe", lambda e, kc=kc: e.matmul(py[:], w[:, kc, o * 128:(o + 1) * 128], hT[:, kc, ts], start=(kc == 0), stop=(kc == 7)),
                         reads=["wmo", ("hT", kc, tt)], writes=[pyk])
                S.op("dve", lambda e: e.scalar_tensor_tensor(out=xT[:, o, ts], in0=py[:], scalar=modT[:, 40 + o:41 + o], in1=xT[:, o, ts],
                                                             op0=ALU.mult, op1=ALU.add),
                     reads=["modT"], writes=[pyk, ("xT", o, tt)])
        S.barrier()


def make_pvB(inp, l, hh):
    pb = np.zeros((128, PB_W), np.float32)
    cs = slice(hh * 128, (hh + 1) * 128)
    pb[:, PB_CW:PB_CW + 31] = np.asarray(inp["conv_w"][l], np.float32)[:, cs].T
    pb[:, PB_CB] = inp["conv_b"][l][cs]
    pb[:, PB_CG] = inp["conv_norm_g"][l][cs]
    pb[:, PB_CBE] = inp["conv_norm_b"][l][cs]
    pb[:, PB_GN] = np.tile(np.asarray(inp["gla_out_norm"][l], np.float32), 2)
    return pb


BF_SEGS = [("QT", 256, TOK), ("KT", 256, TOK), ("GQ", 128, TOK), ("GK", 128, TOK), ("GR", 128, TOK),
           ("V", TOK, 256), ("GKT", TOK, 128), ("GV", TOK, 128)]
F_SEGS = [("KM", 256, 8), ("HC", 128, TOK), ("GE", 128, 32)]
W_SPECS = {"ada_w": [D, 9 * D], "w1i": [D, 2 * DFF], "w1o": [DFF, D], "wmi": [D, INW], "gwb": [17, 256],
           "wmo": [D, D], "w2i": [D, 2 * DFF], "w2o": [DFF, D]}


def _seg_off(segs):
    off, o = {}, 0
    for (n, r, w) in segs:
        off[n] = (o, r, w)
        o += r * w
    return off, o


BF_OFF, NBF = _seg_off(BF_SEGS)
F_OFF, NF = _seg_off(F_SEGS)


def _views(buf_bf, buf_f, n_lead):
    v = {}
    for off, buf in ((BF_OFF, buf_bf), (F_OFF, buf_f)):
        for n, (o, r, w) in off.items():
            v[n] = [buf[i, o:o + r * w].rearrange("(r w) -> r w", w=w) for i in range(n_lead)]
    return v


class XchgA:
    def __init__(self, V):
        self.V = V

    def fm(self, name, blk, ts):
        nb = self.V[name][0].shape[0] // 128
        hs, r0 = blk // nb, (blk % nb) * 128
        return self.V[name][hs][r0:r0 + 128, ts]

    def tm2(self, name, tt, s4, hs):
        t0 = tt * 512 + s4 * 128
        return self.V[name][hs][t0:t0 + 128, :]


class XchgB:
    def __init__(self, V):
        self.V = V

    def fm(self, q, name, r0, nrows, th, tsl):
        return self.V[name][th][r0:r0 + nrows, tsl]

    def tm(self, q, name, th):
        return self.V[name][th]


def _p128(ap):
    return ap.rearrange("(p m) -> p m", p=128)


def build_fused(n_layers=2):
    nc = bass.Bass("TRN2", target_bir_lowering=False, num_devices=8)
    dram = {}
    _decl_in(nc, dram, "xT", [D, TOK], F32)
    _decl_in(nc, dram, "pos", [1, TOK], I32)
    for l in range(n_layers):
        _decl_in(nc, dram, "pv%d" % l, [128, PV_W], F32)
        _decl_in(nc, dram, "pvB%d" % l, [128, PB_W], F32)
        for k, shp in W_SPECS.items():
            _decl_in(nc, dram, "%s%d" % (k, l), shp, F32)
    for k, (shp, dt) in CONST_SPECS.items():
        _decl_in(nc, dram, k, shp, dt)
    out = {}
    _decl_out(nc, out, "yT", [D, TOK], F32)
    XA_bf = [nc.dram_tensor("xa_bf_%d" % l, [2, 2, NBF], BF16, addr_space="Shared").ap() for l in range(n_layers)]
    XA_f = [nc.dram_tensor("xa_f_%d" % l, [2, 2, NF], F32, addr_space="Shared").ap() for l in range(n_layers)]
    XB = [nc.dram_tensor("xb_%d" % l, [2, 2, 512 * TOK], BF16, addr_space="Shared").ap() for l in range(n_layers)]
    LA_bf = nc.dram_tensor("la_bf", [2, NBF], BF16).ap()
    LA_f = nc.dram_tensor("la_f", [2, NF], F32).ap()
    LB_bf = nc.dram_tensor("lb_bf", [2, NBF], BF16).ap()
    LB_f = nc.dram_tensor("lb_f", [2, NF], F32).ap()
    LOT = nc.dram_tensor("lot", [2, 512 * TOK], BF16).ap()
    LC = nc.dram_tensor("lc", [2, 512 * TOK], BF16).ap()
    VA = _views(LA_bf, LA_f, 2)
    VB = _views(LB_bf, LB_f, 2)
    lot_v = [LOT[i, :].rearrange("(r w) -> r w", w=TOK) for i in range(2)]
    lc_v = [LC[i, :].rearrange("(r w) -> r w", w=TOK) for i in range(2)]
    with contextlib.ExitStack() as es:
        S = Sched(nc, es)
        C = Ctx()
        C.banks, C.nxt = _mk_psum(nc, es, S)
        par_sp = nc.sync.snap(nc.sync.partition_id() % 2, min_val=0, max_val=1)
        par_pl = nc.gpsimd.snap(nc.gpsimd.partition_id() % 2, min_val=0, max_val=1)
        xT = es.enter_context(_sbt(nc, "xT_sb", [128, 8, TOK], F32))
        modT = es.enter_context(_sbt(nc, "modT_sb", [128, 72], F32))
        C.acols = es.enter_context(_sbt(nc, "acols", [128, 24], F32))
        C.hg = es.enter_context(_sbt(nc, "hg", [128, 16], F32))
        pvs = [es.enter_context(_sbt(nc, "pv_sb", [128, PV_W], F32)) for l in range(n_layers)]
        pvBs = [es.enter_context(_sbt(nc, "pvB_sb", [128, PB_W], F32)) for l in range(n_layers)]
        xv = dram["xT"].rearrange("(k p) t -> p k t", p=128)
        for k in range(8):
            for tt in range(4):
                S.dma("sp", xT[:, k, tt * 512:(tt + 1) * 512], xv[:, k, tt * 512:(tt + 1) * 512], writes=[("xT", k, tt)])
        for l in range(n_layers):
            S.dma("sp", pvs[l][:], dram["pv%d" % l][:, :], writes=["pv"])
            S.dma("sp", pvBs[l][:], dram["pvB%d" % l][:, :], writes=["pvB"])
        emit_consts_common(S, C, nc, es, dram)
        for l in range(n_layers):
            pv, pvB = pvs[l], pvBs[l]
            W = lambda k: dram["%s%d" % (k, l)]
            with contextlib.ExitStack() as esA:
                hT = esA.enter_context(_sbt(nc, "hT_sb", [128, 8, TOK], BF16))
                emit_adaln(S, C, nc, pv, modT, W("ada_w"))
                emit_mod_cols(S, C, nc, es, pv, modT)
                emit_ffn(S, C, nc, xT, hT,
                         acol=lambda k: C.acols[:, k:k + 1], scol=lambda k: modT[:, k:k + 1], gcol=lambda k: C.hg[:, k:k + 1],
                         w_in=W("w1i"), w_out=W("w1o"), tag="1")
                emit_norm_mod(S, C, xT, hT, acol=lambda k: C.acols[:, 8 + k:9 + k], scol=lambda k: modT[:, 24 + k:25 + k])
                emit_proj(S, C, nc, xT, hT, pv, W("wmi"), dram, XchgA(VA), W("gwb")[:, :], dram["pos"][0:1, :])
                S.barrier()
            S.dma("sp", _p128(XA_bf[l][bass.ds(par_sp, 1)].squeeze(0).rearrange("h n -> (h n)")), _p128(LA_bf.rearrange("h n -> (h n)")))
            S.dma("sp", _p128(XA_f[l][bass.ds(par_sp, 1)].squeeze(0).rearrange("h n -> (h n)")), _p128(LA_f.rearrange("h n -> (h n)")))
            S.barrier()
            nc.all_core_barrier()
            for th in range(2):
                S.dma("pool", _p128(LB_bf[th, :]), _p128(XA_bf[l][th, bass.ds(par_pl, 1), :].squeeze(0)))
                S.dma("pool", _p128(LB_f[th, :]), _p128(XA_f[l][th, bass.ds(par_pl, 1), :].squeeze(0)))
            S.barrier()
            IB = XchgB(VB)

            def OB(r0, nrows, tok0, ntok):
                th, t0 = tok0 // TOK, tok0 % TOK
                return lot_v[th][r0:r0 + nrows, t0:t0 + ntok]

            emit_gla(S, C, nc, pvB, dram, IB, OB)
            C.conv_release = False
            cg = emit_conv(S, C, nc, pvB, dram, IB, OB)
            next(cg)
            emit_attn(S, C, nc, pvB, dram, IB, OB, bg=cg)
            C.conv_release = True
            for _ in cg:
                pass
            S.barrier()
            S.dma("sp", _p128(XB[l][bass.ds(par_sp, 1)].squeeze(0).rearrange("h n -> (h n)")), _p128(LOT.rearrange("h n -> (h n)")))
            S.barrier()
            nc.all_core_barrier()
            for hs in range(2):
                S.dma("pool", _p128(LC[hs, :]), _p128(XB[l][hs, bass.ds(par_pl, 1), :].squeeze(0)))
            S.barrier()
            with contextlib.ExitStack() as esC:
                hT = esC.enter_context(_sbt(nc, "hT_sb", [128, 8, TOK], BF16))

                def IC(k, tsl):
                    return lc_v[k // 4][(k % 4) * 128:(k % 4 + 1) * 128, tsl]

                emit_outproj(S, C, nc, xT, hT, modT, W("wmo"), IC)
                emit_ffn(S, C, nc, xT, hT,
                         acol=lambda k: C.acols[:, 16 + k:17 + k], scol=lambda k: modT[:, 48 + k:49 + k], gcol=lambda k: C.hg[:, 8 + k:9 + k],
                         w_in=W("w2i"), w_out=W("w2o"), tag="2")
        ov = out["yT"].rearrange("(k p) t -> p k t", p=128)
        for k in range(8):
            S.dma("sp", ov[:, k, :], xT[:, k, :], reads=[("xT", k, tt) for tt in range(4)])
        S.finish()
        print("fused: ops", S.nops, "waits", S.nwaits)
    return nc


def host_inputs_fused(inp, consts, n_layers=2):
    maps = []
    shared = {}
    for l in range(n_layers):
        shared["ada_w%d" % l] = np.ascontiguousarray(inp["ada_w"][l], dtype=np.float32)
        shared["w1i%d" % l] = np.ascontiguousarray(inp["ffn1_w_in"][l], dtype=np.float32)
        shared["w1o%d" % l] = np.ascontiguousarray(inp["ffn1_w_out"][l], dtype=np.float32)
        shared["wmi%d" % l] = np.ascontiguousarray(inp["mix_w_in"][l], dtype=np.float32)
        shared["gwb%d" % l] = np.ascontiguousarray(np.concatenate([inp["gla_gate_w"][l], inp["gla_gate_b"][l][None]], axis=0), dtype=np.float32)
        shared["wmo%d" % l] = np.ascontiguousarray(inp["mix_w_out"][l], dtype=np.float32)
        shared["w2i%d" % l] = np.ascontiguousarray(inp["ffn2_w_in"][l], dtype=np.float32)
        shared["w2o%d" % l] = np.ascontiguousarray(inp["ffn2_w_out"][l], dtype=np.float32)
    x = np.asarray(inp["x"], np.float32)
    for r in range(8):
        b, hh = r // 2, r % 2
        m = dict(shared)
        m["xT"] = np.ascontiguousarray(x[b, hh * TOK:(hh + 1) * TOK, :].T)
        m["pos"] = np.ascontiguousarray(inp["positions"][b, hh * TOK:(hh + 1) * TOK][None]).astype(np.int32)
        for l in range(n_layers):
            m["pv%d" % l] = make_pv(inp, l, b)
            m["pvB%d" % l] = make_pvB(inp, l, hh)
        m.update(consts)
        maps.append(m)
    return maps


FUSED = False
_CACHE = {}


def _common_decl(nc, dram, names):
    for k in names:
        _decl_in(nc, dram, k, *CONST_SPECS[k])


def build_uA():
    nc = bass.Bass("TRN2", target_bir_lowering=False)
    dram = {}
    _decl_in(nc, dram, "xT", [D, TOK], F32)
    _decl_in(nc, dram, "pos", [1, TOK], I32)
    _decl_in(nc, dram, "pv", [128, PV_W], F32)
    for k in ("ada_w", "w1i", "w1o", "wmi", "gwb"):
        _decl_in(nc, dram, k, W_SPECS[k], F32)
    _common_decl(nc, dram, ("c_ones", "c_blk", "c_rot", "c_tri"))
    out = {}
    _decl_out(nc, out, "x1T", [D, TOK], F32)
    _decl_out(nc, out, "modT", [128, 72], F32)
    _decl_out(nc, out, "la_bf", [2, NBF], BF16)
    _decl_out(nc, out, "la_f", [2, NF], F32)
    VA = _views(out["la_bf"], out["la_f"], 2)
    with contextlib.ExitStack() as es:
        S = Sched(nc, es)
        C = Ctx()
        C.banks, C.nxt = _mk_psum(nc, es, S)
        xT = es.enter_context(_sbt(nc, "xT_sb", [128, 8, TOK], F32))
        modT = es.enter_context(_sbt(nc, "modT_sb", [128, 72], F32))
        C.acols = es.enter_context(_sbt(nc, "acols", [128, 24], F32))
        C.hg = es.enter_context(_sbt(nc, "hg", [128, 16], F32))
        pv = es.enter_context(_sbt(nc, "pv_sb", [128, PV_W], F32))
        xv = dram["xT"].rearrange("(k p) t -> p k t", p=128)
        for k in range(8):
            for tt in range(4):
                S.dma("sp", xT[:, k, tt * 512:(tt + 1) * 512], xv[:, k, tt * 512:(tt + 1) * 512], writes=[("xT", k, tt)])
        S.dma("sp", pv[:], dram["pv"][:, :], writes=["pv"])
        emit_consts_common(S, C, nc, es, dram)
        with contextlib.ExitStack() as esA:
            hT = esA.enter_context(_sbt(nc, "hT_sb", [128, 8, TOK], BF16))
            emit_adaln(S, C, nc, pv, modT, dram["ada_w"])
            emit_mod_cols(S, C, nc, es, pv, modT)
            S.dma("sp", out["modT"][:, :], modT[:], reads=["modT"])
            emit_ffn(S, C, nc, xT, hT,
                     acol=lambda k: C.acols[:, k:k + 1], scol=lambda k: modT[:, k:k + 1], gcol=lambda k: C.hg[:, k:k + 1],
                     w_in=dram["w1i"], w_out=dram["w1o"], tag="1")
            ov = out["x1T"].rearrange("(k p) t -> p k t", p=128)
            for k in range(8):
                S.dma("sp", ov[:, k, :], xT[:, k, :], reads=[("xT", k, tt) for tt in range(4)])
            emit_norm_mod(S, C, xT, hT, acol=lambda k: C.acols[:, 8 + k:9 + k], scol=lambda k: modT[:, 24 + k:25 + k])
            emit_proj(S, C, nc, xT, hT, pv, dram["wmi"], dram, XchgA(VA), dram["gwb"][:, :], dram["pos"][0:1, :])
        S.finish()
    return nc


def build_uB():
    nc = bass.Bass("TRN2", target_bir_lowering=False)
    dram = {}
    _decl_in(nc, dram, "lb_bf", [2, NBF], BF16)
    _decl_in(nc, dram, "lb_f", [2, NF], F32)
    _decl_in(nc, dram, "pvB", [128, PB_W], F32)
    _common_decl(nc, dram, ("c_onehot", "c_cmask", "c_gmask", "c_bmask", "c_blkf", "c_ident", "c_pastmask", "c_negmask"))
    out = {}
    _decl_out(nc, out, "lot", [2, 512 * TOK], BF16)
    VB = _views(dram["lb_bf"], dram["lb_f"], 2)
    lot_v = [out["lot"][i, :].rearrange("(r w) -> r w", w=TOK) for i in range(2)]
    with contextlib.ExitStack() as es:
        S = Sched(nc, es)
        C = Ctx()
        C.banks, C.nxt = _mk_psum(nc, es, S)
        pvB = es.enter_context(_sbt(nc, "pvB_sb", [128, PB_W], F32))
        C.eps_col = es.enter_context(_sbt(nc, "eps_col", [128, 1], F32))
        C.rs = [es.enter_context(_sbt(nc, "rs%d" % i, [128, 512], F32)) for i in range(2)]
        C.tmp = [es.enter_context(_sbt(nc, "tmp%d" % i, [128, 512], F32)) for i in range(2)]
        S.dma("sp", pvB[:], dram["pvB"][:, :], writes=["pvB"])
        S.op("dve", lambda e: e.memset(C.eps_col[:], EPS), writes=["eps_col"])
        S.barrier()
        IB = XchgB(VB)

        def OB(r0, nrows, tok0, ntok):
            th, t0 = tok0 // TOK, tok0 % TOK
            return lot_v[th][r0:r0 + nrows, t0:t0 + ntok]

        emit_gla(S, C, nc, pvB, dram, IB, OB)
        C.conv_release = False
        cg = emit_conv(S, C, nc, pvB, dram, IB, OB)
        next(cg)
        emit_attn(S, C, nc, pvB, dram, IB, OB, bg=cg)
        C.conv_release = True
        for _ in cg:
            pass
        S.finish()
    return nc


def build_uC():
    nc = bass.Bass("TRN2", target_bir_lowering=False)
    dram = {}
    _decl_in(nc, dram, "x1T", [D, TOK], F32)
    _decl_in(nc, dram, "modT", [128, 72], F32)
    _decl_in(nc, dram, "lc", [2, 512 * TOK], BF16)
    _decl_in(nc, dram, "pv", [128, PV_W], F32)
    for k in ("wmo", "w2i", "w2o"):
        _decl_in(nc, dram, k, W_SPECS[k], F32)
    _common_decl(nc, dram, ("c_ones",))
    out = {}
    _decl_out(nc, out, "x3T", [D, TOK], F32)
    lc_v = [dram["lc"][i, :].rearrange("(r w) -> r w", w=TOK) for i in range(2)]
    with contextlib.ExitStack() as es:
        S = Sched(nc, es)
        C = Ctx()
        C.banks, C.nxt = _mk_psum(nc, es, S)
        xT = es.enter_context(_sbt(nc, "xT_sb", [128, 8, TOK], F32))
        modT = es.enter_context(_sbt(nc, "modT_sb", [128, 72], F32))
        C.acols = es.enter_context(_sbt(nc, "acols", [128, 24], F32))
        C.hg = es.enter_context(_sbt(nc, "hg", [128, 16], F32))
        pv = es.enter_context(_sbt(nc, "pv_sb", [128, PV_W], F32))
        xv = dram["x1T"].rearrange("(k p) t -> p k t", p=128)
        for k in range(8):
            for tt in range(4):
                S.dma("sp", xT[:, k, tt * 512:(tt + 1) * 512], xv[:, k, tt * 512:(tt + 1) * 512], writes=[("xT", k, tt)])
        S.dma("sp", pv[:], dram["pv"][:, :], writes=["pv"])
        S.dma("sp", modT[:], dram["modT"][:, :], writes=["modT"])
        emit_consts_common(S, C, nc, es, dram)
        emit_mod_cols(S, C, nc, es, pv, modT)
        with contextlib.ExitStack() as esC:
            hT = esC.enter_context(_sbt(nc, "hT_sb", [128, 8, TOK], BF16))

            def IC(k, tsl):
                return lc_v[k // 4][(k % 4) * 128:(k % 4 + 1) * 128, tsl]

            emit_outproj(S, C, nc, xT, hT, modT, dram["wmo"], IC)
            emit_ffn(S, C, nc, xT, hT,
                     acol=lambda k: C.acols[:, 16 + k:17 + k], scol=lambda k: modT[:, 48 + k:49 + k], gcol=lambda k: C.hg[:, 8 + k:9 + k],
                     w_in=dram["w2i"], w_out=dram["w2o"], tag="2")
        ov = out["x3T"].rearrange("(k p) t -> p k t", p=128)
        for k in range(8):
            S.dma("sp", ov[:, k, :], xT[:, k, :], reads=[("xT", k, tt) for tt in range(4)])
        S.finish()
    return nc


def kernel_unfused(inp, consts, trace=False):
    if "uA" not in _CACHE:
        _CACHE["uA"], _CACHE["uB"], _CACHE["uC"] = build_uA(), build_uB(), build_uC()
    cores = list(range(8))
    maps = host_inputs_fused(inp, consts, 2)
    xT_cores = [m["xT"] for m in maps]
    times = []
    for l in range(2):
        inA = [{"xT": xT_cores[r], "pos": maps[r]["pos"], "pv": maps[r]["pv%d" % l],
                **{k: maps[r]["%s%d" % (k, l)] for k in ("ada_w", "w1i", "w1o", "wmi", "gwb")},
                **{k: consts[k] for k in ("c_ones", "c_blk", "c_rot", "c_tri")}} for r in cores]
        rA = run_bass_kernel_spmd(_CACHE["uA"], inA, core_ids=cores, trace=trace)
        times.append(("A%d" % l, rA.exec_time_ns))
        rA = rA.results
        inB = [{"lb_bf": np.ascontiguousarray(np.stack([rA[2 * (r // 2) + th]["la_bf"][r % 2] for th in range(2)])),
                "lb_f": np.ascontiguousarray(np.stack([rA[2 * (r // 2) + th]["la_f"][r % 2] for th in range(2)])),
                "pvB": maps[r]["pvB%d" % l],
                **{k: consts[k] for k in ("c_onehot", "c_cmask", "c_gmask", "c_bmask", "c_blkf", "c_ident", "c_pastmask", "c_negmask")}}
               for r in cores]
        rB = run_bass_kernel_spmd(_CACHE["uB"], inB, core_ids=cores, trace=trace)
        times.append(("B%d" % l, rB.exec_time_ns))
        rB = rB.results
        inC = [{"x1T": rA[r]["x1T"], "modT": rA[r]["modT"],
                "lc": np.ascontiguousarray(np.stack([rB[2 * (r // 2) + hs]["lot"][r % 2] for hs in range(2)])),
                "pv": maps[r]["pv%d" % l],
                **{k: maps[r]["%s%d" % (k, l)] for k in ("wmo", "w2i", "w2o")}, "c_ones": consts["c_ones"]} for r in cores]
        rC = run_bass_kernel_spmd(_CACHE["uC"], inC, core_ids=cores, trace=trace)
        times.append(("C%d" % l, rC.exec_time_ns))
        xT_cores = [np.ascontiguousarray(rC.results[r]["x3T"]) for r in cores]
    if trace:
        print("PHASE TIMES", times)
    return xT_cores


def kernel(**inp):
    inp = {k: np.asarray(v) for k, v in inp.items()}
    consts = make_consts()
    out = np.empty((4, SEQ, D), np.float32)
    if FUSED:
        if "nc" not in _CACHE:
            _CACHE["nc"] = build_fused(2)
        res = run_bass_kernel_spmd(_CACHE["nc"], host_inputs_fused(inp, consts, 2), core_ids=list(range(8))).results
        for r in range(8):
            out[r // 2, (r % 2) * TOK:(r % 2 + 1) * TOK, :] = res[r]["yT"].T
    else:
        xs = kernel_unfused(inp, consts)
        for r in range(8):
            out[r // 2, (r % 2) * TOK:(r % 2 + 1) * TOK, :] = xs[r].T
    return out
```
